# Optimizing a Trainium2 kernel written in Bass

```python
import math
import jax, jax.numpy as jnp
from jax import lax
import numpy as np

D_MODEL = 2048
BATCH = 16
SEQ = 2048
DEPTH = 4

CHUNK = 64
MIX_WIDTH = D_MODEL
RET_WIDTH = MIX_WIDTH // 2
RET_HEADS = 4
RET_HEAD_DIM = RET_WIDTH // RET_HEADS
ATT_WIDTH = MIX_WIDTH - RET_WIDTH
ATT_HEAD_DIM = 64
ATT_HEADS = ATT_WIDTH // ATT_HEAD_DIM
N_PREV_CHUNKS = 8
N_BAND_CHUNKS = N_PREV_CHUNKS + 1
BAND = N_BAND_CHUNKS * CHUNK
REL_CLIP = 256
REL_TABLE = REL_CLIP + CHUNK
IN_WIDTH = 4 * RET_WIDTH + 3 * ATT_WIDTH
SPLITS = [RET_WIDTH, 2 * RET_WIDTH, 3 * RET_WIDTH, 4 * RET_WIDTH,
          4 * RET_WIDTH + ATT_WIDTH, 4 * RET_WIDTH + 2 * ATT_WIDTH]
N_EXPERTS = 16
N_GROUPS = 4
EXPERTS_PER_GROUP = N_EXPERTS // N_GROUPS
TOP_K = 2
D_FF = D_MODEL // 2
ROPE_BASE = 10000.0
LN_EPS = 1e-5
DEEPNORM_ALPHA = (2 * DEPTH) ** 0.25
DEEPNORM_BETA = (8 * DEPTH) ** -0.25

kernel_name = "hybrid_retention_chunkattn_grouped_moe_deepnorm"

F32 = jnp.float32


def layer_norm(x, g, b):
    xf = x.astype(F32)
    mu = xf.mean(-1, keepdims=True)
    var = jnp.square(xf - mu).mean(-1, keepdims=True)
    return ((xf - mu) * lax.rsqrt(var + LN_EPS) * g.astype(F32) + b.astype(F32)).astype(x.dtype)


def rotary(t):
    s, d = t.shape[1], t.shape[-1]
    half = d // 2
    inv = ROPE_BASE ** (-jnp.arange(half, dtype=F32) / half)
    ang = jnp.arange(s, dtype=F32)[:, None] * inv[None, :]
    cos = jnp.cos(ang)[None, :, None, :].astype(t.dtype)
    sin = jnp.sin(ang)[None, :, None, :].astype(t.dtype)
    t1, t2 = t[..., :half], t[..., half:]
    return jnp.concatenate([t1 * cos - t2 * sin, t1 * sin + t2 * cos], axis=-1)


def retention(q, k, v, g, norm_gain):
    b, s, h, dk = q.shape
    dv = v.shape[-1]
    nc = s // CHUNK
    dt = q.dtype
    log_gamma = jnp.log1p(-jnp.exp2(-5.0 - jnp.arange(h, dtype=F32)))
    q = rotary(q)
    k = rotary(k) * (dk ** -0.5)
    idx = jnp.arange(CHUNK, dtype=F32)
    intra_decay = jnp.exp(log_gamma[:, None, None] * jnp.abs(idx[:, None] - idx[None, :])).astype(dt)
    key_decay = jnp.exp((CHUNK - 1 - idx)[:, None] * log_gamma[None, :]).astype(dt)
    qry_decay = jnp.exp((idx + 1)[:, None] * log_gamma[None, :]).astype(dt)
    chunk_decay = jnp.exp(log_gamma * CHUNK)
    qc = q.reshape(b, nc, CHUNK, h, dk)
    kc = k.reshape(b, nc, CHUNK, h, dk)
    vc = v.reshape(b, nc, CHUNK, h, dv)
    scores = jnp.einsum('bnqhd,bnkhd->bnhqk', qc, kc) * intra_decay
    intra = jnp.einsum('bnhqk,bnkhe->bnqhe', scores, vc)

    def step(state, xs):
        q_n, k_n, v_n = xs
        cross = jnp.einsum('bqhd,bhde->bqhe', q_n * qry_decay[None, :, :, None], state.astype(dt))
        kv = jnp.einsum('bkhd,bkhe->bhde', k_n * key_decay[None, :, :, None], v_n)
        state = state * chunk_decay[None, :, None, None] + kv.astype(F32)
        return state, cross

    init = jnp.zeros((b, h, dk, dv), F32)
    to_chunk_major = lambda t: jnp.swapaxes(t, 0, 1)
    _, cross = lax.scan(step, init, (to_chunk_major(qc), to_chunk_major(kc), to_chunk_major(vc)))
    o = (intra + to_chunk_major(cross)).reshape(b, s, h, dv)
    of = o.astype(F32)
    mu = of.mean(-1, keepdims=True)
    var = jnp.square(of - mu).mean(-1, keepdims=True)
    on = ((of - mu) * lax.rsqrt(var + LN_EPS)).reshape(b, s, h * dv) * norm_gain.astype(F32)
    return jax.nn.silu(g.reshape(b, s, h * dv)) * on.astype(dt)


def chunk_band_attention(q, k, v, rel_table):
    b, s, h, dh = q.shape
    nc = s // CHUNK
    dt = q.dtype
    qc = q.reshape(b, nc, CHUNK, h, dh)
    pad = ((0, 0), (N_PREV_CHUNKS, 0), (0, 0), (0, 0), (0, 0))
    kp = jnp.pad(k.reshape(b, nc, CHUNK, h, dh), pad)
    vp = jnp.pad(v.reshape(b, nc, CHUNK, h, dh), pad)
    band_idx = np.arange(nc)[:, None] + np.arange(N_BAND_CHUNKS)[None, :]
    kb = kp[:, band_idx].reshape(b, nc, BAND, h, dh)
    vb = vp[:, band_idx].reshape(b, nc, BAND, h, dh)
    scores = jnp.einsum('bnqhd,bnkhd->bnhqk', qc, kb).astype(F32) * (dh ** -0.5)
    dist = N_PREV_CHUNKS * CHUNK + np.arange(CHUNK)[:, None] - np.arange(BAND)[None, :]
    rel_idx = np.clip(dist, -(CHUNK - 1), REL_CLIP) + (CHUNK - 1)
    bias = rel_table[:, rel_idx].astype(F32)
    valid = (np.arange(nc)[:, None] - N_PREV_CHUNKS + np.arange(N_BAND_CHUNKS)[None, :]) >= 0
    valid = np.repeat(valid, CHUNK, axis=1)
    scores = scores + bias[None, None]
    scores = jnp.where(valid[None, :, None, None, :], scores, jnp.finfo(F32).min)
    p = jax.nn.softmax(scores, axis=-1).astype(dt)
    o = jnp.einsum('bnhqk,bnkhd->bnqhd', p, vb)
    return o.reshape(b, s, h * dh)


def grouped_top2_moe(x, router_w, router_b, w_gate, w_up, w_down):
    b, s, d = x.shape
    dt = x.dtype
    t = x.reshape(b * s, d)
    affinity = jax.nn.sigmoid(jnp.dot(t, router_w).astype(F32))
    sel = affinity + router_b.astype(F32)[None, :]
    grp_score = lax.top_k(sel.reshape(-1, N_GROUPS, EXPERTS_PER_GROUP), TOP_K)[0].sum(-1)
    grp_choice = jnp.argmax(grp_score, axis=-1)
    expert_group = jnp.arange(N_EXPERTS) // EXPERTS_PER_GROUP
    in_group = expert_group[None, :] == grp_choice[:, None]
    _, top_idx = lax.top_k(jnp.where(in_group, sel, -jnp.inf), TOP_K)
    top_aff = jnp.take_along_axis(affinity, top_idx, axis=-1)
    top_w = top_aff / top_aff.sum(-1, keepdims=True)
    combine = jnp.sum(jax.nn.one_hot(top_idx, N_EXPERTS, dtype=F32) * top_w[..., None], axis=1).astype(dt)
    y = jnp.zeros_like(t)
    for e in range(N_EXPERTS):
        hdn = jax.nn.silu(jnp.dot(t, w_gate[e])) * jnp.dot(t, w_up[e])
        y = y + combine[:, e:e + 1] * jnp.dot(hdn, w_down[e])
    return y.reshape(b, s, d)


def setup_inputs(seed: int = 0) -> dict:
    key = jax.random.key(seed)
    ks = jax.random.split(key, 16)
    nrm = lambda k, shape: jax.random.normal(k, shape, F32)
    x = nrm(ks[0], (BATCH, SEQ, D_MODEL))
    col_scale = jnp.concatenate([
        jnp.ones((2 * RET_WIDTH,), F32), jnp.full((RET_WIDTH,), DEEPNORM_BETA, F32),
        jnp.ones((RET_WIDTH + 2 * ATT_WIDTH,), F32), jnp.full((ATT_WIDTH,), DEEPNORM_BETA, F32)])
    w_in = nrm(ks[1], (DEPTH, D_MODEL, IN_WIDTH)) * (D_MODEL ** -0.5) * col_scale
    ret_norm_gain = 1.0 + 0.02 * nrm(ks[2], (DEPTH, RET_WIDTH))
    rel_bias = 0.1 * nrm(ks[3], (DEPTH, ATT_HEADS, REL_TABLE))
    w_out = nrm(ks[4], (DEPTH, MIX_WIDTH, D_MODEL)) * (MIX_WIDTH ** -0.5) * DEEPNORM_BETA
    ln1_g = 1.0 + 0.02 * nrm(ks[5], (DEPTH, D_MODEL))
    ln1_b = 0.02 * nrm(ks[6], (DEPTH, D_MODEL))
    router_w = nrm(ks[7], (D_MODEL, N_EXPERTS)) * (D_MODEL ** -0.5)
    router_b = 0.01 * nrm(ks[8], (N_EXPERTS,))
    w_gate = nrm(ks[9], (DEPTH, N_EXPERTS, D_MODEL, D_FF)) * (D_MODEL ** -0.5)
    w_up = nrm(ks[10], (DEPTH, N_EXPERTS, D_MODEL, D_FF)) * (D_MODEL ** -0.5) * DEEPNORM_BETA
    w_down = nrm(ks[11], (DEPTH, N_EXPERTS, D_FF, D_MODEL)) * (D_FF ** -0.5) * DEEPNORM_BETA
    ln2_g = 1.0 + 0.02 * nrm(ks[12], (DEPTH, D_MODEL))
    ln2_b = 0.02 * nrm(ks[13], (DEPTH, D_MODEL))
    return {"x": x, "w_in": w_in, "ret_norm_gain": ret_norm_gain, "rel_bias": rel_bias,
            "w_out": w_out, "ln1_g": ln1_g, "ln1_b": ln1_b, "router_w": router_w,
            "router_b": router_b, "w_gate": w_gate, "w_up": w_up, "w_down": w_down,
            "ln2_g": ln2_g, "ln2_b": ln2_b}


def reference(x, w_in, ret_norm_gain, rel_bias, w_out, ln1_g, ln1_b, router_w, router_b,
              w_gate, w_up, w_down, ln2_g, ln2_b):
    b, s, _ = x.shape
    for l in range(DEPTH):
        proj = jnp.dot(x, w_in[l])
        rq, rk, rv, rg, aq, ak, av = jnp.split(proj, SPLITS, axis=-1)
        rh = lambda t: t.reshape(b, s, RET_HEADS, RET_HEAD_DIM)
        ah = lambda t: t.reshape(b, s, ATT_HEADS, ATT_HEAD_DIM)
        ret_out = retention(rh(rq), rh(rk), rh(rv), rh(rg), ret_norm_gain[l])
        att_out = chunk_band_attention(ah(aq), ah(ak), ah(av), rel_bias[l])
        mixed = jnp.concatenate([ret_out, att_out], axis=-1)
        x = layer_norm(DEEPNORM_ALPHA * x + jnp.dot(mixed, w_out[l]), ln1_g[l], ln1_b[l])
        moe = grouped_top2_moe(x, router_w, router_b, w_gate[l], w_up[l], w_down[l])
        x = layer_norm(DEEPNORM_ALPHA * x + moe, ln2_g[l], ln2_b[l])
    return x
```

```python
import contextlib
import numpy as np
import ml_dtypes
import concourse.bass as bass
import concourse.mybir as mybir
from concourse.bass_utils import run_bass_kernel_spmd

F32 = mybir.dt.float32
BF16 = mybir.dt.bfloat16
AF = mybir.ActivationFunctionType
ALU = mybir.AluOpType
AX = mybir.AxisListType

D = 2048
NE = 16
INW = 7168
ALPHA = 8.0 ** 0.25
EPS = 1e-5
FULL = dict(L=4, NSEQ=2, S=2048, DFF=1024, NCORES=8)
SAME_ENG_SYNC = True


class Sem:
    def __init__(self, h):
        self.h = h
        self.n = 0


class Res:
    __slots__ = ("w", "r", "excl")

    def __init__(self, excl=False):
        self.w = {}
        self.r = {}
        self.excl = excl


class Eng:
    def __init__(self, e, sem, is_pe=False):
        self.e = e
        self.sem = sem
        self.is_pe = is_pe
        self.waited = {}


class Tracker:
    def __init__(self):
        self.all_sems = []

    def _wait(self, eng, reads, writes):
        d = {}
        for r in reads:
            for s, t in r.w.items():
                if d.get(s, 0) < t:
                    d[s] = t
            if r.excl:
                for s, t in r.r.items():
                    if d.get(s, 0) < t:
                        d[s] = t
        for w in writes:
            for s, t in w.w.items():
                if d.get(s, 0) < t:
                    d[s] = t
            for s, t in w.r.items():
                if d.get(s, 0) < t:
                    d[s] = t
        for s, t in d.items():
            if s is eng.sem and (eng.is_pe or not SAME_ENG_SYNC):
                continue
            if eng.waited.get(s, 0) < t:
                eng.e.wait_ge(s.h, t)
                eng.waited[s] = t

    @staticmethod
    def _mark(sem, tick, reads, writes):
        for r in reads:
            if r.excl:
                r.w = {sem: tick}
                r.r = {}
            elif r.r.get(sem, 0) < tick:
                r.r[sem] = tick
        for w in writes:
            w.w = {sem: tick}
            w.r = {}

    def op(self, eng, fn, reads=(), writes=(), signal=True):
        self._wait(eng, reads, writes)
        ins = fn()
        if signal:
            eng.sem.n += 1
            ins.then_inc(eng.sem.h, 1)
            tick = eng.sem.n
        else:
            tick = eng.sem.n + 1
        self._mark(eng.sem, tick, reads, writes)

    def dma(self, q, sem, pairs, reads=(), writes=()):
        self._wait(q, reads, writes)
        for out, in_ in pairs:
            q.e.dma_start(out=out, in_=in_).then_inc(sem.h, 16)
            sem.n += 16
        self._mark(sem, sem.n, reads, writes)

    def barrier(self, engs):
        for e in engs:
            for s in self.all_sems:
                if s.n > 0 and e.waited.get(s, 0) < s.n and not (s is e.sem):
                    e.e.wait_ge(s.h, s.n)
                    e.waited[s] = s.n


def build_program(cfg):
    L, NSEQ, S, DFF = cfg["L"], cfg["NSEQ"], cfg["S"], cfg["DFF"]
    T = NSEQ * S
    NT = S // 128
    NFT = DFF // 128
    nc = bass.Bass("TRN2", target_bir_lowering=False)
    es = contextlib.ExitStack()

    def din(name, shape, dt=F32):
        return nc.dram_tensor(name, list(shape), dt, kind="ExternalInput").ap()

    def dint(name, shape, dt):
        return nc.dram_tensor(name, list(shape), dt, kind="Internal").ap()

    x_in = din("x", [T, D])
    w_in = din("w_in", [L * D, INW])
    w_out = din("w_out", [L * D, D])
    w_gate = din("w_gate", [L * NE * D, DFF])
    w_up = din("w_up", [L * NE * D, DFF])
    w_down = din("w_down", [L * NE * DFF, D])
    bias_exp = din("bias_exp", [L * 16 * 128, 640])
    ret_gain = din("ret_gain", [L, 1024])
    ln1_g = din("ln1_g", [L, D]); ln1_b = din("ln1_b", [L, D])
    ln2_g = din("ln2_g", [L, D]); ln2_b = din("ln2_b", [L, D])
    router_w = din("router_w", [D, NE])
    router_b = din("router_b", [1, NE])
    c_cos = din("c_cos", [128, S]); c_sin = din("c_sin", [128, S])
    c_dmask = din("c_dmask", [128, 4 * 128]); c_qdec = din("c_qdec", [128, 4 * 128])
    c_kdec = din("c_kdec", [128, 4])
    c_identb = din("c_identb", [128, 128], BF16); c_identf = din("c_identf", [128, 128])
    c_maskneg = din("c_maskneg", [128, 640])
    c_sel = din("c_sel", [16, 16 * 128])
    y_out = nc.dram_tensor("y", [T, D], F32, kind="ExternalOutput").ap()

    Win16s = [dint(f"Win16_{i}", [28 * 128, 4096], BF16) for i in range(2)]
    Wout16s = [dint(f"Wout16_{i}", [8 * 128, 4096], BF16) for i in range(2)]
    Wgu16s = [dint(f"Wgu16_{i}", [NE * NFT * 128, 4096], BF16) for i in range(2)]
    Wd16s = [dint(f"Wd16_{i}", [NE * 4 * 128, NFT * 512], BF16) for i in range(2)]
    Xa = dint("Xa", [T, D], F32); Xb = dint("Xb", [T, D], F32)
    Ta = dint("Ta", [16, 128, T], BF16); Tb = dint("Tb", [16, 128, T], BF16)

    trk = Tracker()

    def newsem(name):
        s = Sem(es.enter_context(nc.semaphore(name)))
        trk.all_sems.append(s)
        return s

    PE = Eng(nc.tensor, newsem("s_pe"), is_pe=True)
    ACT = Eng(nc.scalar, newsem("s_act"))
    DVE = Eng(nc.vector, newsem("s_dve"))
    POOL = Eng(nc.gpsimd, newsem("s_pool"))
    SP = Eng(nc.sync, newsem("s_sp"))
    ENGS = [PE, ACT, DVE, POOL, SP]

    uid = [0]

    def sb(stack, name, shape, dt):
        uid[0] += 1
        return stack.enter_context(nc.sbuf_tensor(f"{name}_{uid[0]}", list(shape), dt))

    identb = sb(es, "identb", [128, 128], BF16)
    identf = sb(es, "identf", [128, 128], F32)
    dmask = sb(es, "dmask", [128, 4, 128], F32)
    qdecB = sb(es, "qdecB", [128, 4, 128], F32)
    kdec = sb(es, "kdec", [128, 4], F32)
    rw16 = sb(es, "rw16", [128, 16, NE], BF16)
    rbB = sb(es, "rbB", [128, NE], F32)
    epsT = sb(es, "epsT", [128, 1], F32)
    r_const = Res()

    pbanks = [es.enter_context(nc.psum_tensor(f"pb{i}", [128, 512], F32)) for i in range(7)]
    pbT = pbanks[6] if cfg.get("NOPBT") else es.enter_context(nc.psum_tensor("pbT", [128, 512], F32))
    _pb = [Res(excl=True) for _ in range(8)]
    pres = [[_pb[i]] * 4 for i in range(7)]
    pTres = [_pb[7]] * 4

    sem_c = newsem("s_const")
    dsem = [newsem(f"s_d{i}") for i in range(20)]

    trk.dma(SP, sem_c, [(identb[:], c_identb[:, :]), (identf[:], c_identf[:, :]),
                        (dmask[:].rearrange("p h q -> p (h q)"), c_dmask[:, :]),
                        (qdecB[:].rearrange("p h q -> p (h q)"), c_qdec[:, :]),
                        (kdec[:], c_kdec[:, :]),
                        (rbB[:], router_b[0].partition_broadcast(128))], writes=[r_const])
    trk.op(DVE, lambda: nc.vector.memset(epsT[:], EPS), writes=[r_const])
    with contextlib.ExitStack() as st0:
        rwf = sb(st0, "rwf", [128, 16, NE], F32)
        r_rwf = Res()
        trk.dma(SP, sem_c, [(rwf[:], router_w.rearrange("(k p) e -> p k e", p=128))], writes=[r_rwf])
        trk.op(DVE, lambda: nc.vector.tensor_copy(rw16[:], rwf[:]), reads=[r_rwf], writes=[r_const])
        trk.barrier(ENGS)

    def tres(n):
        return [Res() for _ in range(n)]
    R_Xa, R_Xb, R_Ta, R_Tb, R_y = tres(T // 128), tres(T // 128), tres(T // 128), tres(T // 128), tres(T // 128)
    R_xin = tres(T // 128)
    R_w16s = [{k: Res() for k in ("in", "out", "gu", "d")} for _ in range(2)]

    cast_rr = [0]

    def cast_items(l):
        si = l % 2
        Win16, Wout16, Wgu16, Wd16, R_w16 = Win16s[si], Wout16s[si], Wgu16s[si], Wd16s[si], R_w16s[si]
        items = []
        wi = w_in[l * D:(l + 1) * D, :]
        for g in range(28):
            for hf in range(2):
                items.append(([(0, (8, 256), wi[hf * 1024:(hf + 1) * 1024, g * 256:(g + 1) * 256].rearrange("(k p) c -> p k c", p=128))],
                              2048, Win16[g * 128:(g + 1) * 128, hf * 2048:(hf + 1) * 2048], R_w16["in"]))
        wo = w_out[l * D:(l + 1) * D, :]
        for g in range(8):
            for hf in range(2):
                items.append(([(0, (8, 256), wo[hf * 1024:(hf + 1) * 1024, g * 256:(g + 1) * 256].rearrange("(k p) c -> p k c", p=128))],
                              2048, Wout16[g * 128:(g + 1) * 128, hf * 2048:(hf + 1) * 2048], R_w16["out"]))
        for e in range(NE):
            r0 = (l * NE + e) * D
            for ft in range(NFT):
                ti = e * NFT + ft
                for hf, wsrc in enumerate((w_gate, w_up)):
                    items.append(([(0, (16, 128), wsrc[r0:r0 + D, ft * 128:(ft + 1) * 128].rearrange("(k p) c -> p k c", p=128))],
                                  2048, Wgu16[ti * 128:(ti + 1) * 128, hf * 2048:(hf + 1) * 2048], R_w16["gu"]))
        nh = 2 if NFT >= 2 else 1
        fh = NFT // nh
        for e in range(NE):
            r0 = (l * NE + e) * DFF
            for dq in range(4):
                ti = e * 4 + dq
                for hf in range(nh):
                    items.append(([(0, (fh, 512), w_down[r0 + hf * fh * 128:r0 + (hf + 1) * fh * 128, dq * 512:(dq + 1) * 512].rearrange("(k p) c -> p k c", p=128))],
                                  fh * 512, Wd16[ti * 128:(ti + 1) * 128, hf * fh * 512:(hf + 1) * fh * 512], R_w16["d"]))
        return items

    class CastPump:
        def __init__(self, items, f32b, b16b, sem0):
            self.items, self.f32b, self.b16b, self.sem0 = items, f32b, b16b, sem0
            self.rf = [Res(), Res()]
            self.rb = [Res(), Res()]
            self.i = 0

        def done(self):
            return self.i - 2 >= len(self.items)

        def pump(self):
            i, items = self.i, self.items
            if self.done():
                return
            if 0 <= i - 1 < len(items):
                k = (i - 1) % 2
                n = items[i - 1][1]
                trk.op(POOL, lambda: nc.gpsimd.tensor_copy(self.b16b[k][:, 0:n], self.f32b[k][:, 0:n]), reads=[self.rf[k]], writes=[self.rb[k]])
            if 0 <= i - 2 < len(items):
                k = (i - 2) % 2
                _, n, dst, res = items[i - 2]
                trk.dma(SP, dsem[self.sem0 + 2 + k], [(dst, self.b16b[k][:, 0:n])], reads=[self.rb[k]], writes=[res])
            if i < len(items):
                k = i % 2
                srcs = items[i][0]
                pairs = [(self.f32b[k][:, off:off + a_ * b_].rearrange("p (a b) -> p a b", b=b_), src) for off, (a_, b_), src in srcs]
                trk.dma(SP, dsem[self.sem0 + k], pairs, writes=[self.rf[k]])
            self.i += 1

    def cast_layer_now(l):
        with contextlib.ExitStack() as st:
            f32b = [sb(st, f"cf{i}", [128, 2048], F32) for i in range(2)]
            b16b = [sb(st, f"cb{i}", [128, 2048], BF16) for i in range(2)]
            cp = CastPump(cast_items(l), f32b, b16b, 16)
            while not cp.done():
                cp.pump()
            trk.barrier(ENGS)

    def layer_norm_tile(ytile, ry, gB, bB, stats, mv, sc, rs):
        for c in range(4):
            trk.op(DVE, lambda c=c: nc.vector.bn_stats(stats[:, c, :], ytile[:, c * 512:(c + 1) * 512]), reads=[ry], writes=[rs])
        trk.op(DVE, lambda: nc.vector.bn_aggr(mv[:, 0:2], stats[:].rearrange("p c s -> p (c s)")), reads=[rs], writes=[rs])
        trk.op(ACT, lambda: nc.scalar.activation(sc[:, 0:1], mv[:, 1:2], AF.Sqrt, bias=epsT[:, 0:1], scale=1.0), reads=[rs, r_const], writes=[rs])
        trk.op(DVE, lambda: nc.vector.reciprocal(sc[:, 0:1], sc[:, 0:1]), reads=[rs], writes=[rs])
        trk.op(DVE, lambda: nc.vector.scalar_tensor_tensor(sc[:, 1:2], mv[:, 0:1], -1.0, sc[:, 0:1], ALU.mult, ALU.mult), reads=[rs], writes=[rs])
        trk.op(ACT, lambda: nc.scalar.activation(ytile[:], ytile[:], AF.Identity, bias=sc[:, 1:2], scale=sc[:, 0:1]), reads=[rs, ry], writes=[ry])
        trk.op(POOL, lambda: nc.gpsimd.tensor_tensor(ytile[:], ytile[:], gB[:], ALU.mult), reads=[ry, r_const], writes=[ry])
        trk.op(DVE, lambda: nc.vector.tensor_tensor(ytile[:], ytile[:], bB[:], ALU.add), reads=[ry, r_const], writes=[ry])

    tp_rr = [0]

    def transpose_tile_to_xT(src16, rsrc, dstT, col0, rdst):
        for g in range(8):
            half = tp_rr[0] % 2
            tp_rr[0] += 1
            for j in range(2):
                kc = g * 2 + j
                trk.op(PE, lambda kc=kc, j=j, half=half: nc.tensor.matmul(
                    pbT[:, half * 256 + j * 128: half * 256 + (j + 1) * 128], src16[:, kc * 128:(kc + 1) * 128], identb[:], start=True, stop=True),
                    reads=[rsrc, r_const], writes=[pTres[half * 2 + j]], signal=(j == 1))
            rr = [pTres[half * 2 + j] for j in range(2)]
            outv = dstT[:, g * 2:(g + 1) * 2, col0:col0 + 128]
            inv = pbT[:, half * 256:(half + 1) * 256].rearrange("p (j t) -> p j t", j=2)
            if g % 2 == 0:
                trk.op(ACT, lambda outv=outv, inv=inv: nc.scalar.copy(outv, inv), reads=rr, writes=[rdst])
            else:
                trk.op(DVE, lambda outv=outv, inv=inv: nc.vector.tensor_copy(outv, inv), reads=rr, writes=[rdst])

    def Tview(Tbuf, t0, n):
        return Tbuf[:, :, t0:t0 + n].rearrange("k p t -> p k t")

    def phase0():
        with contextlib.ExitStack() as st:
            xf = [sb(st, f"p0x{i}", [128, D], F32) for i in range(2)]
            xb = [sb(st, f"p0b{i}", [128, D], BF16) for i in range(2)]
            xT = [sb(st, f"p0t{i}", [128, 16, 128], BF16) for i in range(2)]
            rxf = [Res(), Res()]; rxb = [Res(), Res()]; rxT = [Res(), Res()]
            for t in range(T // 128):
                k = t % 2
                trk.dma(SP, dsem[k], [(xf[k][:], x_in[t * 128:(t + 1) * 128, :])], reads=[R_xin[t]], writes=[rxf[k]])
                P0 = cfg.get("P0", 9)
                if P0 >= 2:
                    trk.op(POOL, lambda k=k: nc.gpsimd.tensor_copy(xb[k][:], xf[k][:]), reads=[rxf[k]], writes=[rxb[k]])
                if P0 >= 3:
                    transpose_tile_to_xT(xb[k], rxb[k], xT[k], 0, rxT[k])
                if P0 >= 4:
                    trk.dma(SP, dsem[2 + k], [(Tview(Ta, t * 128, 128), xT[k][:])], reads=[rxT[k]], writes=[R_Ta[t]])
            trk.barrier(ENGS)

    def phaseA(l, Xsrc, R_Xsrc):
        with contextlib.ExitStack() as st:
            BLK = 256
            g1B = sb(st, "g1B", [128, D], F32); b1B = sb(st, "b1B", [128, D], F32)
            gainB = sb(st, "gainB", [128, 1024], F32)
            biasT = sb(st, "biasT", [128, 16, 640], BF16)
            trk.dma(SP, sem_c, [(g1B[:], ln1_g[l].partition_broadcast(128)), (b1B[:], ln1_b[l].partition_broadcast(128)),
                                (gainB[:], ret_gain[l].partition_broadcast(128))], writes=[r_const])
            with contextlib.ExitStack() as st2:
                mneg = sb(st2, "mneg", [128, 640], F32)
                bf = [sb(st2, f"bf{i}", [128, 640], F32) for i in range(2)]
                rbf = [Res(), Res()]
                trk.dma(SP, sem_c, [(mneg[:], c_maskneg[:, :])], writes=[r_const])
                for h in range(16):
                    k = h % 2
                    r0 = (l * 16 + h) * 128
                    trk.dma(SP, dsem[8 + k], [(bf[k][:], bias_exp[r0:r0 + 128, :])], writes=[rbf[k]])
                    trk.op(DVE, lambda h=h, k=k: nc.vector.tensor_tensor(biasT[:, h, :], bf[k][:], mneg[:], ALU.add),
                           reads=[rbf[k], r_const], writes=[r_const])
                trk.barrier(ENGS)

            xTb = [sb(st, f"xTb{i}", [128, 16, BLK], BF16) for i in range(2)]
            rxTb = [Res(), Res()]
            x32 = [sb(st, f"x32_{i}", [128, D], F32) for i in range(2)]
            rx32 = [Res(), Res()]
            cs = [sb(st, f"cs{i}", [128, 2, BLK], F32) for i in range(2)]
            rcs = [Res(), Res()]
            NW = 3
            wp = [sb(st, f"wp{i}", [128, 16, 256], BF16) for i in range(NW)]
            rwp = [Res() for _ in range(NW)]
            qkT = sb(st, "qkT", [128, 16, BLK], BF16)
            rqk = [Res() for _ in range(8)]
            qTa = sb(st, "qTa", [128, 8, BLK], BF16)
            rqa = [Res() for _ in range(4)]
            RING = 6
            Kring = sb(st, "Kring", [128, 8, RING * 128], BF16)
            Vring = sb(st, "Vring", [128, RING, 16, 65], BF16)
            rK = [[Res() for _ in range(4)] for _ in range(RING)]
            rV = [[Res() for _ in range(4)] for _ in range(RING)]
            vret = sb(st, "vret", [128, 2, 1024], BF16)
            rvret = [[Res() for _ in range(4)] for _ in range(2)]
            sg = sb(st, "sg", [128, 2, 1024], BF16)
            rsg = [[Res() for _ in range(4)] for _ in range(2)]
            mixed = sb(st, "mixed", [128, D], BF16)
            rmixed = Res()
            mixedT = sb(st, "mixedT", [128, 16, BLK], BF16)
            rmT = [Res(), Res()]
            state = sb(st, "state", [128, 4, 2, 256], F32)
            state16 = sb(st, "state16", [128, 4, 2, 256], BF16)
            rstate = [Res() for _ in range(4)]
            t1s = sb(st, "t1s", [128, BLK], F32); t2s = sb(st, "t2s", [128, BLK], F32)
            ra = sb(st, "ra", [128, BLK], F32); rbt = sb(st, "rbt", [128, BLK], F32)
            rc = sb(st, "rc", [128, BLK], F32); rd = sb(st, "rd", [128, BLK], F32)
            rrot = Res(); rrot2 = Res()
            PTs = [sb(st, f"PT{i}", [128, 128], BF16) for i in range(2)]
            rPT = [Res(), Res()]
            Es = [sb(st, f"E{i}", [128, 128], BF16) for i in range(6)]
            rE = [Res() for _ in range(6)]
            qd = [sb(st, f"qd{i}", [128, 2, 128], BF16) for i in range(2)]
            rqd = [Res(), Res()]
            ktd = [sb(st, f"ktd{i}", [128, 256], BF16) for i in range(2)]
            rktd = [Res(), Res()]
            onb = [sb(st, f"on{i}", [128, 256], F32) for i in range(2)]
            ron = [Res(), Res()]
            stats = sb(st, "stats", [128, 4, 6], F32); mv = sb(st, "mv", [128, 4], F32); sc = sb(st, "sc", [128, 4], F32)
            rs = Res()
            hst = [sb(st, f"hst{i}", [128, 8], F32) for i in range(2)]
            rhst = [Res(), Res()]
            xn16 = sb(st, "xn16", [128, D], BF16); rxn = Res()
            xTs = sb(st, "xTs", [128, 16, BLK], BF16); rxTs = Res()

            trk.op(POOL, lambda: nc.gpsimd.memset(Vring[:].rearrange("p r h e -> p (r h e)"), 1.0), writes=[rV[i][j] for i in range(RING) for j in range(4)])

            wcnt = [0]
            ecnt = [0]
            pcnt = {"acc": 0, "sc": 0, "o": 0, "kv": 0, "ao": 0}

            def load_w(src, r0, c0):
                k = wcnt[0] % NW
                wcnt[0] += 1
                g_ = c0 // 256
                trk.dma(SP, dsem[6 + k], [(wp[k][:].rearrange("p k c -> p (k c)"), src[g_ * 128:(g_ + 1) * 128, :])],
                        reads=[R_w16s[l % 2]["in"], R_w16s[l % 2]["out"]], writes=[rwp[k]])
                return wp[k], rwp[k]

            nblk = S // BLK
            for s in range(NSEQ):
                for h in range(4):
                    trk.op(POOL, lambda h=h: nc.gpsimd.memset(state[:, h].rearrange("p a b -> p (a b)"), 0.0), writes=[rstate[h]])
                    trk.op(POOL, lambda h=h: nc.gpsimd.memset(state16[:, h].rearrange("p a b -> p (a b)"), 0.0), writes=[rstate[h]])
                for b in range(nblk):
                    tok0 = s * S + b * BLK
                    gt0 = tok0 // 128
                    kb = b % 2
                    trk.dma(SP, dsem[kb], [(xTb[kb][:], Tview(Ta, tok0, BLK))], reads=[R_Ta[gt0], R_Ta[gt0 + 1]], writes=[rxTb[kb]])
                    trk.dma(SP, dsem[2 + kb], [(cs[kb][:, 0, :], c_cos[:, b * BLK:(b + 1) * BLK]), (cs[kb][:, 1, :], c_sin[:, b * BLK:(b + 1) * BLK])], writes=[rcs[kb]])
                    xt = xTb[kb]
                    for g in range(28):
                        wt, rw = load_w(Win16s[l % 2], 0, g * 256)
                        kind = g // 4
                        if kind in (0, 1, 4, 5):
                            bank = pcnt["acc"] % 2
                            pcnt["acc"] += 1
                            for j in range(2):
                                for kc in range(16):
                                    trk.op(PE, lambda j=j, kc=kc: nc.tensor.matmul(pbanks[bank][:, j * 256:(j + 1) * 256], wt[:, kc, j * 128:(j + 1) * 128], xt[:, kc, :],
                                                                                  start=(kc == 0), stop=(kc == 15)),
                                           reads=[rw, rxTb[kb]], writes=[pres[bank][2 * j], pres[bank][2 * j + 1]], signal=(kc == 15))
                            pr = pres[bank]
                            if kind in (0, 1):
                                hh = g % 4
                                scl = 1.0 if kind == 0 else 1.0 / 16.0
                                trk.op(ACT, lambda: nc.scalar.activation(t1s[:], pbanks[bank][:, 0:256], AF.Copy, scale=scl), reads=[pr[0], pr[1]], writes=[rrot])
                                trk.op(ACT, lambda: nc.scalar.activation(t2s[:], pbanks[bank][:, 256:512], AF.Copy, scale=scl), reads=[pr[2], pr[3]], writes=[rrot])
                                cosv = cs[kb][:, 0, :]; sinv = cs[kb][:, 1, :]
                                o1 = qkT[:, kind * 8 + 2 * hh, :]; o2 = qkT[:, kind * 8 + 2 * hh + 1, :]
                                rq = rqk[kind * 4 + hh]
                                trk.op(DVE, lambda: nc.vector.tensor_tensor(ra[:], t1s[:], cosv, ALU.mult), reads=[rrot, rcs[kb]], writes=[rrot2])
                                trk.op(POOL, lambda: nc.gpsimd.tensor_tensor(rbt[:], t2s[:], sinv, ALU.mult), reads=[rrot, rcs[kb]], writes=[rrot2])
                                trk.op(DVE, lambda: nc.vector.tensor_tensor(rc[:], t1s[:], sinv, ALU.mult), reads=[rrot, rcs[kb]], writes=[rrot2])
                                trk.op(POOL, lambda: nc.gpsimd.tensor_tensor(rd[:], t2s[:], cosv, ALU.mult), reads=[rrot, rcs[kb]], writes=[rrot2])
                                trk.op(DVE, lambda: nc.vector.tensor_tensor(o1, ra[:], rbt[:], ALU.subtract), reads=[rrot2], writes=[rq])
                                trk.op(POOL, lambda: nc.gpsimd.tensor_tensor(o2, rc[:], rd[:], ALU.add), reads=[rrot2], writes=[rq])
                            elif kind == 4:
                                gg = g % 4
                                trk.op(ACT, lambda: nc.scalar.activation(qTa[:, 2 * gg:2 * gg + 2, :], pbanks[bank][:].rearrange("p (j t) -> p j t", j=2), AF.Copy, scale=0.125),
                                       reads=pr, writes=[rqa[gg]])
                            else:
                                gg = g % 4
                                for tt in range(2):
                                    slot = (2 * b + tt) % RING
                                    trk.op(DVE, lambda tt=tt, slot=slot: nc.vector.tensor_copy(
                                        Kring[:, 2 * gg:2 * gg + 2, slot * 128:(slot + 1) * 128],
                                        pbanks[bank][:].rearrange("p (j t) -> p j t", j=2)[:, :, tt * 128:(tt + 1) * 128]),
                                        reads=pr, writes=[rK[slot][gg]])
                        else:
                            gg = g % 4
                            for tt in range(2):
                                bank = pcnt["acc"] % 2
                                pcnt["acc"] += 1
                                for kc in range(16):
                                    trk.op(PE, lambda kc=kc, tt=tt: nc.tensor.matmul(pbanks[bank][:, 0:256], xt[:, kc, tt * 128:(tt + 1) * 128], wt[:, kc, :],
                                                                                  start=(kc == 0), stop=(kc == 15)),
                                           reads=[rw, rxTb[kb]], writes=[pres[bank][0], pres[bank][1]], signal=(kc == 15))
                                pr = [pres[bank][0], pres[bank][1]]
                                if kind == 2:
                                    trk.op(ACT, lambda tt=tt: nc.scalar.copy(vret[:, tt, gg * 256:(gg + 1) * 256], pbanks[bank][:, 0:256]), reads=pr, writes=[rvret[tt][gg]])
                                elif kind == 3:
                                    trk.op(ACT, lambda tt=tt: nc.scalar.activation(onb[tt][:], pbanks[bank][:, 0:256], AF.Silu), reads=pr, writes=[ron[tt]])
                                    trk.op(POOL, lambda tt=tt: nc.gpsimd.tensor_tensor(sg[:, tt, gg * 256:(gg + 1) * 256], onb[tt][:], gainB[:, gg * 256:(gg + 1) * 256], ALU.mult),
                                           reads=[ron[tt], r_const], writes=[rsg[tt][gg]])
                                else:
                                    slot = (2 * b + tt) % RING
                                    trk.op(DVE, lambda tt=tt, slot=slot: nc.vector.tensor_copy(
                                        Vring[:, slot, 4 * gg:4 * gg + 4, 0:64], pbanks[bank][:, 0:256].rearrange("p (h e) -> p h e", h=4)),
                                        reads=pr, writes=[rV[slot][gg]])
                    for tt in range(2):
                        tis = 2 * b + tt
                        slot = tis % RING
                        cols = slice(tt * 128, (tt + 1) * 128)
                        for h in range(4):
                            kq = pcnt["sc"] % 8; pcnt["sc"] += 1
                            sbank, sreg = 2 + kq % 2, kq // 2
                            sps = pbanks[sbank][:, sreg * 128:(sreg + 1) * 128]
                            rsp = pres[sbank][sreg]
                            for dc in range(2):
                                trk.op(PE, lambda dc=dc: nc.tensor.matmul(sps, qkT[:, 8 + 2 * h + dc, cols], qkT[:, 2 * h + dc, cols], start=(dc == 0), stop=(dc == 1)),
                                       reads=[rqk[4 + h], rqk[h]], writes=[rsp], signal=(dc == 1))
                            kp = ecnt[0] % 2; ecnt[0] += 1
                            trk.op(DVE, lambda: nc.vector.tensor_tensor(PTs[kp][:], sps, dmask[:, h, :], ALU.mult), reads=[rsp, r_const], writes=[rPT[kp]])
                            for dc in range(2):
                                trk.op(POOL, lambda dc=dc: nc.gpsimd.tensor_tensor(qd[kp][:, dc, :], qkT[:, 2 * h + dc, cols], qdecB[:, h, :], ALU.mult),
                                       reads=[rqk[h], r_const], writes=[rqd[kp]])
                            ko = pcnt["o"] % 2; pcnt["o"] += 1
                            ops = pbanks[4][:, ko * 256:(ko + 1) * 256]
                            rop = [pres[4][2 * ko], pres[4][2 * ko + 1]]
                            vv = vret[:, tt, h * 256:(h + 1) * 256]
                            trk.op(PE, lambda: nc.tensor.matmul(ops, PTs[kp][:], vv, start=True, stop=False), reads=[rPT[kp], rvret[tt][h]], writes=rop, signal=False)
                            for dc in range(2):
                                trk.op(PE, lambda dc=dc: nc.tensor.matmul(ops, qd[kp][:, dc, :], state16[:, h, dc, :], start=False, stop=(dc == 1)),
                                       reads=[rqd[kp], rstate[h]], writes=rop, signal=(dc == 1))
                            kh = ecnt[0] % 2
                            trk.op(DVE, lambda: nc.vector.bn_stats(hst[kh][:, 0:6], ops), reads=rop, writes=[rhst[kh]])
                            trk.op(DVE, lambda: nc.vector.bn_aggr(hst[kh][:, 6:8], hst[kh][:, 0:6]), reads=[rhst[kh]], writes=[rhst[kh]])
                            trk.op(ACT, lambda: nc.scalar.activation(hst[kh][:, 0:1], hst[kh][:, 7:8], AF.Sqrt, bias=epsT[:, 0:1], scale=1.0), reads=[rhst[kh], r_const], writes=[rhst[kh]])
                            trk.op(DVE, lambda: nc.vector.reciprocal(hst[kh][:, 0:1], hst[kh][:, 0:1]), reads=[rhst[kh]], writes=[rhst[kh]])
                            trk.op(DVE, lambda: nc.vector.scalar_tensor_tensor(hst[kh][:, 1:2], hst[kh][:, 6:7], -1.0, hst[kh][:, 0:1], ALU.mult, ALU.mult), reads=[rhst[kh]], writes=[rhst[kh]])
                            trk.op(ACT, lambda: nc.scalar.activation(onb[kh][:], ops, AF.Identity, bias=hst[kh][:, 1:2], scale=hst[kh][:, 0:1]), reads=rop + [rhst[kh]], writes=[ron[kh]])
                            trk.op(POOL, lambda: nc.gpsimd.tensor_tensor(mixed[:, h * 256:(h + 1) * 256], onb[kh][:], sg[:, tt, h * 256:(h + 1) * 256], ALU.mult),
                                   reads=[ron[kh], rsg[tt][h]], writes=[rmixed])
                            half = tp_rr[0] % 2; tp_rr[0] += 1
                            for dc in range(2):
                                trk.op(PE, lambda dc=dc: nc.tensor.matmul(pbT[:, half * 256 + dc * 128: half * 256 + (dc + 1) * 128], qkT[:, 8 + 2 * h + dc, cols], identb[:], start=True, stop=True),
                                       reads=[rqk[4 + h], r_const], writes=[pTres[half * 2 + dc]], signal=(dc == 1))
                            trk.op(ACT, lambda: nc.scalar.activation(ktd[kp][:], pbT[:, half * 256: half * 256 + 256], AF.Identity, scale=kdec[:, h:h + 1]),
                                   reads=[pTres[half * 2], pTres[half * 2 + 1], r_const], writes=[rktd[kp]])
                            for dc in range(2):
                                kk = pcnt["kv"] % 2; pcnt["kv"] += 1
                                kps = pbanks[5][:, kk * 256:(kk + 1) * 256]
                                rkp = [pres[5][2 * kk], pres[5][2 * kk + 1]]
                                trk.op(PE, lambda dc=dc: nc.tensor.matmul(kps, ktd[kp][:, dc * 128:(dc + 1) * 128], vv, start=True, stop=True), reads=[rktd[kp], rvret[tt][h]], writes=rkp)
                                g128 = float(np.exp(128.0 * np.log1p(-2.0 ** (-5.0 - h))))
                                trk.op(DVE, lambda dc=dc: nc.vector.scalar_tensor_tensor(state[:, h, dc, :], state[:, h, dc, :], g128, kps, ALU.mult, ALU.add), reads=rkp + [rstate[h]], writes=[rstate[h]])
                                trk.op(POOL, lambda dc=dc: nc.gpsimd.tensor_copy(state16[:, h, dc, :], state[:, h, dc, :]), reads=[rstate[h]], writes=[rstate[h]])
                        kts = [kt for kt in range(5) if tis - 4 + kt >= 0]
                        for h in range(16):
                            hp, pb0 = h // 2, 64 * (h % 2)
                            ecur = []
                            for kt in kts:
                                kslot = (tis - 4 + kt) % RING
                                kq = pcnt["sc"] % 8; pcnt["sc"] += 1
                                sbank, sreg = 2 + kq % 2, kq // 2
                                sps = pbanks[sbank][:, sreg * 128:(sreg + 1) * 128]
                                rsp = pres[sbank][sreg]
                                trk.op(PE, lambda: nc.tensor.matmul(sps, Kring[pb0:pb0 + 64, hp, kslot * 128:(kslot + 1) * 128], qTa[pb0:pb0 + 64, hp, cols], start=True, stop=False),
                                       reads=[rK[kslot][hp // 2], rqa[hp // 2]], writes=[rsp], signal=False)
                                trk.op(PE, lambda: nc.tensor.matmul(sps, identb[:], biasT[:, h, kt * 128:(kt + 1) * 128], start=False, stop=True), reads=[r_const], writes=[rsp])
                                ke = ecnt[0] % 6; ecnt[0] += 1
                                trk.op(ACT, lambda: nc.scalar.activation(Es[ke][:], sps, AF.Exp), reads=[rsp], writes=[rE[ke]])
                                ecur.append((ke, kslot))
                            ka = pcnt["ao"] % 4; pcnt["ao"] += 1
                            aps = pbanks[6][:, ka * 128: ka * 128 + 65]
                            rap = pres[6][ka]
                            for i, (ke, kslot) in enumerate(ecur):
                                trk.op(PE, lambda: nc.tensor.matmul(aps, Es[ke][:], Vring[:, kslot, h, :], start=(i == 0), stop=(i == len(ecur) - 1)),
                                       reads=[rE[ke], rV[kslot][h // 4]], writes=[rap], signal=(i == len(ecur) - 1))
                            kh = ecnt[0] % 2
                            trk.op(DVE, lambda: nc.vector.reciprocal(hst[kh][:, 0:1], pbanks[6][:, ka * 128 + 64: ka * 128 + 65]), reads=[rap], writes=[rhst[kh]])
                            trk.op(DVE, lambda: nc.vector.tensor_scalar(mixed[:, 1024 + h * 64: 1024 + (h + 1) * 64], pbanks[6][:, ka * 128: ka * 128 + 64], hst[kh][:, 0:1], None, ALU.mult),
                                   reads=[rap, rhst[kh]], writes=[rmixed])
                        transpose_tile_to_xT(mixed, rmixed, mixedT, tt * 128, rmT[tt])
                    wts = [load_w(Wout16s[l % 2], 0, g * 256) for g in range(3)]
                    for tt in range(2):
                        gt = gt0 + tt
                        kx = tt
                        trk.dma(SP, dsem[4 + kx], [(x32[kx][:], Xsrc[gt * 128:(gt + 1) * 128, :])], reads=[R_Xsrc[gt]], writes=[rx32[kx]])
                    for g in range(8):
                        if g < 3:
                            wt, rw = wts[g]
                        else:
                            wt, rw = load_w(Wout16s[l % 2], 0, g * 256)
                        for tt in range(2):
                            bank = pcnt["acc"] % 2; pcnt["acc"] += 1
                            for mc in range(16):
                                trk.op(PE, lambda mc=mc, tt=tt: nc.tensor.matmul(pbanks[bank][:, 0:256], mixedT[:, mc, tt * 128:(tt + 1) * 128], wt[:, mc, :], start=(mc == 0), stop=(mc == 15)),
                                       reads=[rw, rmT[tt]], writes=[pres[bank][0], pres[bank][1]], signal=(mc == 15))
                            yv = x32[tt][:, g * 256:(g + 1) * 256]
                            trk.op(DVE, lambda yv=yv: nc.vector.scalar_tensor_tensor(yv, yv, ALPHA, pbanks[bank][:, 0:256], ALU.mult, ALU.add),
                                   reads=[pres[bank][0], pres[bank][1], rx32[tt]], writes=[rx32[tt]])
                    for tt in range(2):
                        gt = gt0 + tt
                        layer_norm_tile(x32[tt], rx32[tt], g1B, b1B, stats, mv, sc, rs)
                        trk.dma(SP, dsem[10 + tt], [(Xb[gt * 128:(gt + 1) * 128, :], x32[tt][:])], reads=[rx32[tt]], writes=[R_Xb[gt]])
                        trk.op(ACT, lambda tt=tt: nc.scalar.copy(xn16[:], x32[tt][:]), reads=[rx32[tt]], writes=[rxn])
                        transpose_tile_to_xT(xn16, rxn, xTs, tt * 128, rxTs)
                    trk.dma(SP, dsem[12], [(Tview(Tb, tok0, BLK), xTs[:])], reads=[rxTs], writes=[R_Tb[gt0], R_Tb[gt0 + 1]])
            trk.barrier(ENGS)

    def phaseB(l, Xdst, R_Xdst, last):
        with contextlib.ExitStack() as st:
            BLK = 512
            Wgu16, Wd16, R_w16 = Wgu16s[l % 2], Wd16s[l % 2], R_w16s[l % 2]
            cp = None
            if not last and cfg.get("OVERLAP_CAST", 1):
                cf32 = [sb(st, f"pcf{i}", [128, 2048], F32) for i in range(2)]
                cb16 = [sb(st, f"pcb{i}", [128, 2048], BF16) for i in range(2)]
                cp = CastPump(cast_items(l + 1), cf32, cb16, 16)
            g2B = sb(st, "g2B", [128, D], F32); b2B = sb(st, "b2B", [128, D], F32)
            selT = sb(st, "selT", [16, 16, 128], F32)
            trk.dma(SP, sem_c, [(g2B[:], ln2_g[l].partition_broadcast(128)), (b2B[:], ln2_b[l].partition_broadcast(128)),
                                (selT[:].rearrange("p e q -> p (e q)"), c_sel[:, :])], writes=[r_const])
            xTb = [sb(st, f"bxT{i}", [128, 16, BLK], BF16) for i in range(2)]
            rxTb = [Res(), Res()]
            acc = [sb(st, f"acc{i}", [128, D], F32) for i in range(4)]
            racc = [Res() for _ in range(4)]
            NG = 2
            gu = [sb(st, f"gu{i}", [128, 2, 16, 128], BF16) for i in range(NG)]
            rgu = [Res() for _ in range(NG)]
            ND = 2
            dw = [sb(st, f"dw{i}", [128, NFT, 512], BF16) for i in range(ND)]
            rdw = [Res() for _ in range(ND)]
            hT = [sb(st, f"hT{i}", [128, NFT, BLK], BF16) for i in range(2)]
            rhT = [Res(), Res()]
            cbB = sb(st, "cbB", [128, NE, BLK], BF16); rcbB = Res()
            cbT = sb(st, "cbT", [16, BLK], F32); rcbT = Res()
            sil = [sb(st, f"sil{i}", [128, BLK], F32) for i in range(2)]
            rsil = [Res(), Res()]
            sa = [sb(st, f"sa{i}", [128, BLK], F32) for i in range(2)]
            rsa = [Res(), Res()]
            rt = sb(st, "rt", [128, 160], F32); rrt = Res()
            stats = sb(st, "bstats", [128, 4, 6], F32); mv = sb(st, "bmv", [128, 4], F32); sc = sb(st, "bsc", [128, 4], F32)
            rs = Res()
            xn16 = sb(st, "bxn16", [128, D], BF16); rxn = Res()
            xTs = sb(st, "bxTs", [128, 16, 256], BF16); rxTs = Res()
            gcnt = [0]; dcnt = [0]; cnt = {"g": 0, "d": 0, "s": 0}

            for bi in range(T // BLK):
                tok0 = bi * BLK
                gt0 = tok0 // 128
                kb = bi % 2
                trk.dma(SP, dsem[kb], [(xTb[kb][:], Tview(Tb, tok0, BLK))], reads=[R_Tb[gt0 + i] for i in range(4)], writes=[rxTb[kb]])
                xt = xTb[kb]
                for tt in range(4):
                    trk.dma(SP, dsem[2 + tt], [(acc[tt][:], Xb[(gt0 + tt) * 128:(gt0 + tt + 1) * 128, :])], reads=[R_Xb[gt0 + tt]], writes=[racc[tt]])
                for tt in range(4):
                    lg = pbanks[6][:, 0:16]
                    for kc in range(16):
                        trk.op(PE, lambda kc=kc: nc.tensor.matmul(lg, xt[:, kc, tt * 128:(tt + 1) * 128], rw16[:, kc, :], start=(kc == 0), stop=(kc == 15)),
                               reads=[rxTb[kb], r_const], writes=[pres[6][0]], signal=(kc == 15))
                    aff = rt[:, 0:16]; sel = rt[:, 16:32]; ps6 = rt[:, 32:56]; grp = rt[:, 56:60]; gmx = rt[:, 60:61]
                    eqg = rt[:, 61:65]; msk = rt[:, 65:81]; t1 = rt[:, 81:82]; eq1 = rt[:, 82:98]; m2 = rt[:, 98:114]
                    t2 = rt[:, 114:115]; eq2 = rt[:, 115:131]; wsel = rt[:, 131:147]; den = rt[:, 147:148]; cb = rt[:, 148:164] if False else None
                    V = nc.vector
                    R = dict(reads=[rrt], writes=[rrt])
                    trk.op(ACT, lambda: nc.scalar.activation(aff, lg, AF.Sigmoid), reads=[pres[6][0]], writes=[rrt])
                    trk.op(DVE, lambda: V.tensor_tensor(sel, aff, rbB[:], ALU.add), reads=[rrt, r_const], writes=[rrt])
                    sel3 = sel.rearrange("p (g e) -> p g e", e=4)
                    ps3 = ps6.rearrange("p (g c) -> p g c", c=6)
                    for idx, (i, j) in enumerate([(0, 1), (0, 2), (0, 3), (1, 2), (1, 3), (2, 3)]):
                        trk.op(DVE, lambda idx=idx, i=i, j=j: V.tensor_tensor(ps3[:, :, idx], sel3[:, :, i], sel3[:, :, j], ALU.add), **R)
                    trk.op(DVE, lambda: V.tensor_reduce(grp, ps3, AX.X, ALU.max), **R)
                    trk.op(DVE, lambda: V.tensor_reduce(gmx, grp, AX.X, ALU.max), **R)
                    trk.op(DVE, lambda: V.tensor_scalar(eqg, grp, gmx, None, ALU.is_equal), **R)
                    msk3 = msk.rearrange("p (g e) -> p g e", e=4)
                    for e in range(4):
                        trk.op(DVE, lambda e=e: V.scalar_tensor_tensor(msk3[:, :, e], sel3[:, :, e], 10.0, eqg, ALU.add, ALU.mult), **R)
                    trk.op(DVE, lambda: V.tensor_reduce(t1, msk, AX.X, ALU.max), **R)
                    trk.op(DVE, lambda: V.tensor_scalar(eq1, msk, t1, None, ALU.is_equal), **R)
                    trk.op(DVE, lambda: V.scalar_tensor_tensor(m2, eq1, -100.0, msk, ALU.mult, ALU.add), **R)
                    trk.op(DVE, lambda: V.tensor_reduce(t2, m2, AX.X, ALU.max), **R)
                    trk.op(DVE, lambda: V.tensor_scalar(eq2, m2, t2, None, ALU.is_equal), **R)
                    trk.op(DVE, lambda: V.tensor_tensor(eq1, eq1, eq2, ALU.add), **R)
                    trk.op(DVE, lambda: V.tensor_tensor(wsel, aff, eq1, ALU.mult), **R)
                    trk.op(DVE, lambda: V.tensor_reduce(den, wsel, AX.X, ALU.add), **R)
                    trk.op(DVE, lambda: V.reciprocal(den, den), **R)
                    trk.op(DVE, lambda: V.tensor_scalar(wsel, wsel, den, None, ALU.mult), **R)
                    trk.op(PE, lambda: nc.tensor.matmul(pbanks[6][0:16, 128:256], wsel, identf[:], start=True, stop=True), reads=[rrt, r_const], writes=[pres[6][1]])
                    trk.op(ACT, lambda tt=tt: nc.scalar.copy(cbT[:, tt * 128:(tt + 1) * 128], pbanks[6][0:16, 128:256]), reads=[pres[6][1]], writes=[rcbT])
                for e in range(NE):
                    trk.op(PE, lambda e=e: nc.tensor.matmul(pbanks[6][:, :], selT[:, e, :], cbT[:, :], start=True, stop=True), reads=[rcbT, r_const], writes=pres[6])
                    trk.op(ACT, lambda e=e: nc.scalar.copy(cbB[:, e, :], pbanks[6][:, :]), reads=pres[6], writes=[rcbB])
                for e in range(NE):
                    kh = e % 2
                    for ft in range(NFT):
                        if cp is not None:
                            cp.pump()
                        k = gcnt[0] % NG; gcnt[0] += 1
                        ti = e * NFT + ft
                        trk.dma(SP, dsem[6 + k], [(gu[k][:].rearrange("p a k c -> p (a k c)"), Wgu16[ti * 128:(ti + 1) * 128, :])],
                                reads=[R_w16["gu"]], writes=[rgu[k]])
                        gb = cnt["g"] % 2; cnt["g"] += 1
                        for which in range(2):
                            bank = gb * 2 + which
                            for kc in range(16):
                                trk.op(PE, lambda kc=kc, which=which, bank=bank: nc.tensor.matmul(pbanks[bank][:, :], gu[k][:, which, kc, :], xt[:, kc, :], start=(kc == 0), stop=(kc == 15)),
                                       reads=[rgu[k], rxTb[kb]], writes=pres[bank], signal=(kc == 15))
                        ks = cnt["s"] % 2; cnt["s"] += 1
                        trk.op(ACT, lambda: nc.scalar.activation(sil[ks][:], pbanks[gb * 2][:, :], AF.Silu), reads=pres[gb * 2], writes=[rsil[ks]])
                        trk.op(POOL, lambda: nc.gpsimd.tensor_tensor(sa[ks][:], sil[ks][:], cbB[:, e, :], ALU.mult), reads=[rsil[ks], rcbB], writes=[rsa[ks]])
                        trk.op(DVE, lambda: nc.vector.tensor_tensor(hT[kh][:, ft, :], sa[ks][:], pbanks[gb * 2 + 1][:, :], ALU.mult), reads=[rsa[ks]] + pres[gb * 2 + 1], writes=[rhT[kh]])
                    for dq in range(4):
                        k = dcnt[0] % ND; dcnt[0] += 1
                        ti = e * 4 + dq
                        trk.dma(SP, dsem[8 + k], [(dw[k][:].rearrange("p k c -> p (k c)"), Wd16[ti * 128:(ti + 1) * 128, :])],
                                reads=[R_w16["d"]], writes=[rdw[k]])
                        for tt in range(4):
                            bank = 4 + cnt["d"] % 2; cnt["d"] += 1
                            for ft in range(NFT):
                                trk.op(PE, lambda ft=ft, tt=tt, bank=bank: nc.tensor.matmul(pbanks[bank][:, :], hT[kh][:, ft, tt * 128:(tt + 1) * 128], dw[k][:, ft, :], start=(ft == 0), stop=(ft == NFT - 1)),
                                       reads=[rhT[kh], rdw[k]], writes=pres[bank], signal=(ft == NFT - 1))
                            av = acc[tt][:, dq * 512:(dq + 1) * 512]
                            if e == 0:
                                trk.op(DVE, lambda av=av, bank=bank: nc.vector.scalar_tensor_tensor(av, av, ALPHA, pbanks[bank][:, :], ALU.mult, ALU.add), reads=pres[bank] + [racc[tt]], writes=[racc[tt]])
                            else:
                                trk.op(DVE, lambda av=av, bank=bank: nc.vector.tensor_tensor(av, av, pbanks[bank][:, :], ALU.add), reads=pres[bank] + [racc[tt]], writes=[racc[tt]])
                for tt in range(4):
                    gt = gt0 + tt
                    layer_norm_tile(acc[tt], racc[tt], g2B, b2B, stats, mv, sc, rs)
                    trk.dma(SP, dsem[10 + tt], [(Xdst[gt * 128:(gt + 1) * 128, :], acc[tt][:])], reads=[racc[tt]], writes=[R_Xdst[gt]])
                    if not last:
                        trk.op(ACT, lambda tt=tt: nc.scalar.copy(xn16[:], acc[tt][:]), reads=[racc[tt]], writes=[rxn])
                        transpose_tile_to_xT(xn16, rxn, xTs, (tt % 2) * 128, rxTs)
                        if tt % 2 == 1:
                            trk.dma(SP, dsem[14], [(Tview(Ta, tok0 + (tt - 1) * 128, 256), xTs[:])], reads=[rxTs], writes=[R_Ta[gt - 1], R_Ta[gt]])
            if cp is not None:
                while not cp.done():
                    cp.pump()
            trk.barrier(ENGS)

    STOP = cfg.get("STOP", 99)
    if STOP >= 0:
        phase0()
    for l in range(L):
        if STOP <= 0:
            break
        if l == 0 or not cfg.get("OVERLAP_CAST", 1):
            cast_layer_now(l)
        if STOP <= 1:
            break
        if l == 0:
            phaseA(l, x_in, R_xin)
        else:
            phaseA(l, Xa, R_Xa)
        last = (l == L - 1)
        if STOP <= 2:
            break
        phaseB(l, y_out if last else Xa, R_y if last else R_Xa, last)
    trk.barrier(ENGS)
    es.close()
    return nc


def host_constants(cfg):
    S = cfg["S"]
    half = 128
    inv = (10000.0 ** (-np.arange(half, dtype=np.float32) / half)).astype(np.float32)
    ang = (np.arange(S, dtype=np.float32)[None, :] * inv[:, None]).astype(np.float32)
    c = {}
    c["c_cos"] = np.cos(ang).astype(np.float32)
    c["c_sin"] = np.sin(ang).astype(np.float32)
    lg = np.log1p(-np.exp2(-5.0 - np.arange(4, dtype=np.float32))).astype(np.float32)
    k = np.arange(128)[:, None]; q = np.arange(128)[None, :]
    dm = np.zeros((128, 4, 128), np.float32)
    for h in range(4):
        m = np.exp(lg[h] * np.abs(q - k).astype(np.float32)).astype(np.float32)
        m = np.where((k // 64) <= (q // 64), m, 0.0)
        dm[:, h, :] = m
    c["c_dmask"] = dm.reshape(128, 512)
    qd = np.zeros((128, 4, 128), np.float32)
    kd = np.zeros((128, 4), np.float32)
    for h in range(4):
        qd[:, h, :] = np.exp(lg[h] * (np.arange(128, dtype=np.float32) + 1.0))[None, :]
        kd[:, h] = np.exp(lg[h] * (127.0 - np.arange(128, dtype=np.float32)))
    c["c_qdec"] = qd.reshape(128, 512)
    c["c_kdec"] = kd
    c["c_identb"] = np.eye(128, dtype=np.float32).astype(ml_dtypes.bfloat16)
    c["c_identf"] = np.eye(128, dtype=np.float32)
    mneg = np.zeros((128, 5, 128), np.float32)
    mneg[64:128, 4, 0:64] = -30000.0
    mneg[0:64, 0, 64:128] = -30000.0
    c["c_maskneg"] = mneg.reshape(128, 640)
    sel = np.zeros((16, 16, 128), np.float32)
    for e in range(16):
        sel[e, e, :] = 1.0
    c["c_sel"] = sel.reshape(16, 2048)
    return c


def bias_index():
    k = np.arange(128)[:, None, None]; kt = np.arange(5)[None, :, None]; q = np.arange(128)[None, None, :]
    dist = q - k + 512 - 128 * kt
    return (np.clip(dist, -63, 256) + 63).reshape(128 * 640)


_CACHE = {}


def run(cfg, inputs, trace=False):
    L, NSEQ, S, DFF, NCORES = cfg["L"], cfg["NSEQ"], cfg["S"], cfg["DFF"], cfg["NCORES"]
    key = tuple(sorted(cfg.items()))
    if key not in _CACHE:
        _CACHE[key] = build_program(cfg)
    nc = _CACHE[key]
    f = lambda a: np.ascontiguousarray(np.asarray(a, dtype=np.float32))
    x = f(inputs["x"])
    shared = dict(
        w_in=f(inputs["w_in"]).reshape(L * D, INW),
        w_out=f(inputs["w_out"]).reshape(L * D, D),
        w_gate=f(inputs["w_gate"]).reshape(L * NE * D, DFF),
        w_up=f(inputs["w_up"]).reshape(L * NE * D, DFF),
        w_down=f(inputs["w_down"]).reshape(L * NE * DFF, D),
        bias_exp=np.ascontiguousarray(np.take(f(inputs["rel_bias"]), bias_index(), axis=2).reshape(L * 16 * 128, 640)),
        ret_gain=f(inputs["ret_norm_gain"]),
        ln1_g=f(inputs["ln1_g"]), ln1_b=f(inputs["ln1_b"]), ln2_g=f(inputs["ln2_g"]), ln2_b=f(inputs["ln2_b"]),
        router_w=f(inputs["router_w"]), router_b=f(inputs["router_b"]).reshape(1, NE),
    )
    shared.update(host_constants(cfg))
    in_maps = []
    for c in range(NCORES):
        m = dict(shared)
        m["x"] = np.ascontiguousarray(x[c * NSEQ:(c + 1) * NSEQ].reshape(NSEQ * S, D))
        in_maps.append(m)
    res = run_bass_kernel_spmd(nc, in_maps, core_ids=list(range(NCORES)), **({"trace": True} if trace else {}))
    out = np.stack([r["y"].reshape(NSEQ, S, D) for r in res.results], 0).reshape(NCORES * NSEQ, S, D)
    return out.astype(np.float32), res


def kernel(x, w_in, ret_norm_gain, rel_bias, w_out, ln1_g, ln1_b, router_w, router_b,
           w_gate, w_up, w_down, ln2_g, ln2_b):
    inputs = dict(x=x, w_in=w_in, ret_norm_gain=ret_norm_gain, rel_bias=rel_bias, w_out=w_out,
                  ln1_g=ln1_g, ln1_b=ln1_b, router_w=router_w, router_b=router_b,
                  w_gate=w_gate, w_up=w_up, w_down=w_down, ln2_g=ln2_g, ln2_b=ln2_b)
    out, _ = run(FULL, inputs)
    return out
```

```python
import contextlib
import numpy as np
import ml_dtypes
import concourse.bass as bass
import concourse.mybir as mybir
from concourse.bass_utils import run_bass_kernel_spmd

F32 = mybir.dt.float32
BF16 = mybir.dt.bfloat16
AF = mybir.ActivationFunctionType
ALU = mybir.AluOpType
AX = mybir.AxisListType

D = 2048
NE = 16
INW = 7168
ALPHA = 8.0 ** 0.25
EPS = 1e-5
FULL = dict(L=4, NSEQ=2, S=2048, DFF=1024, NCORES=8)
SAME_ENG_SYNC = True


class Sem:
    def __init__(self, h):
        self.h = h
        self.n = 0


class Res:
    __slots__ = ("w", "r", "excl")

    def __init__(self, excl=False):
        self.w = {}
        self.r = {}
        self.excl = excl


class Eng:
    def __init__(self, e, sem, is_pe=False):
        self.e = e
        self.sem = sem
        self.is_pe = is_pe
        self.waited = {}


class Tracker:
    def __init__(self):
        self.all_sems = []

    def _wait(self, eng, reads, writes):
        d = {}
        for r in reads:
            for s, t in r.w.items():
                if d.get(s, 0) < t:
                    d[s] = t
            if r.excl:
                for s, t in r.r.items():
                    if d.get(s, 0) < t:
                        d[s] = t
        for w in writes:
            for s, t in w.w.items():
                if d.get(s, 0) < t:
                    d[s] = t
            for s, t in w.r.items():
                if d.get(s, 0) < t:
                    d[s] = t
        for s, t in d.items():
            if s is eng.sem and (eng.is_pe or not SAME_ENG_SYNC):
                continue
            if eng.waited.get(s, 0) < t:
                eng.e.wait_ge(s.h, t)
                eng.waited[s] = t

    @staticmethod
    def _mark(sem, tick, reads, writes):
        for r in reads:
            if r.excl:
                r.w = {sem: tick}
                r.r = {}
            elif r.r.get(sem, 0) < tick:
                r.r[sem] = tick
        for w in writes:
            w.w = {sem: tick}
            w.r = {}

    def op(self, eng, fn, reads=(), writes=(), signal=True):
        self._wait(eng, reads, writes)
        ins = fn()
        if signal:
            eng.sem.n += 1
            ins.then_inc(eng.sem.h, 1)
            tick = eng.sem.n
        else:
            tick = eng.sem.n + 1
        self._mark(eng.sem, tick, reads, writes)

    def dma(self, q, sem, pairs, reads=(), writes=()):
        self._wait(q, reads, writes)
        for out, in_ in pairs:
            q.e.dma_start(out=out, in_=in_).then_inc(sem.h, 16)
            sem.n += 16
        self._mark(sem, sem.n, reads, writes)

    def barrier(self, engs):
        for e in engs:
            for s in self.all_sems:
                if s.n > 0 and e.waited.get(s, 0) < s.n and not (s is e.sem):
                    e.e.wait_ge(s.h, s.n)
                    e.waited[s] = s.n


def build_program(cfg):
    L, NSEQ, S, DFF = cfg["L"], cfg["NSEQ"], cfg["S"], cfg["DFF"]
    T = NSEQ * S
    NT = S // 128
    NFT = DFF // 128
    nc = bass.Bass("TRN2", target_bir_lowering=False)
    es = contextlib.ExitStack()

    def din(name, shape, dt=F32):
        return nc.dram_tensor(name, list(shape), dt, kind="ExternalInput").ap()

    def dint(name, shape, dt):
        return nc.dram_tensor(name, list(shape), dt, kind="Internal").ap()

    x_in = din("x", [T, D])
    w_in = din("w_in", [L * D, INW])
    w_out = din("w_out", [L * D, D])
    w_gate = din("w_gate", [L * NE * D, DFF])
    w_up = din("w_up", [L * NE * D, DFF])
    w_down = din("w_down", [L * NE * DFF, D])
    bias_exp = din("bias_exp", [L * 16 * 128, 640])
    ret_gain = din("ret_gain", [L, 1024])
    ln1_g = din("ln1_g", [L, D]); ln1_b = din("ln1_b", [L, D])
    ln2_g = din("ln2_g", [L, D]); ln2_b = din("ln2_b", [L, D])
    router_w = din("router_w", [D, NE])
    router_b = din("router_b", [1, NE])
    c_cos = din("c_cos", [128, S]); c_sin = din("c_sin", [128, S])
    c_dmask = din("c_dmask", [128, 4 * 128]); c_qdec = din("c_qdec", [128, 4 * 128])
    c_kdec = din("c_kdec", [128, 4])
    c_identb = din("c_identb", [128, 128], BF16); c_identf = din("c_identf", [128, 128])
    c_maskneg = din("c_maskneg", [128, 640])
    c_sel = din("c_sel", [16, 16 * 128])
    y_out = nc.dram_tensor("y", [T, D], F32, kind="ExternalOutput").ap()

    Win16s = [dint(f"Win16_{i}", [28 * 128, 4096], BF16) for i in range(2)]
    Wout16s = [dint(f"Wout16_{i}", [8 * 128, 4096], BF16) for i in range(2)]
    Wgu16s = [dint(f"Wgu16_{i}", [NE * NFT * 128, 4096], BF16) for i in range(2)]
    Wd16s = [dint(f"Wd16_{i}", [NE * 4 * 128, NFT * 512], BF16) for i in range(2)]
    Xa = dint("Xa", [T, D], F32); Xb = dint("Xb", [T, D], F32)
    Ta = dint("Ta", [16, 128, T], BF16); Tb = dint("Tb", [16, 128, T], BF16)

    trk = Tracker()

    def newsem(name):
        s = Sem(es.enter_context(nc.semaphore(name)))
        trk.all_sems.append(s)
        return s

    PE = Eng(nc.tensor, newsem("s_pe"), is_pe=True)
    ACT = Eng(nc.scalar, newsem("s_act"))
    DVE = Eng(nc.vector, newsem("s_dve"))
    POOL = Eng(nc.gpsimd, newsem("s_pool"))
    SP = Eng(nc.sync, newsem("s_sp"))
    ENGS = [PE, ACT, DVE, POOL, SP]

    uid = [0]

    def sb(stack, name, shape, dt):
        uid[0] += 1
        return stack.enter_context(nc.sbuf_tensor(f"{name}_{uid[0]}", list(shape), dt))

    identb = sb(es, "identb", [128, 128], BF16)
    identf = sb(es, "identf", [128, 128], F32)
    dmask = sb(es, "dmask", [128, 4, 128], F32)
    qdecB = sb(es, "qdecB", [128, 4, 128], F32)
    kdec = sb(es, "kdec", [128, 4], F32)
    rw16 = sb(es, "rw16", [128, 16, NE], BF16)
    rbB = sb(es, "rbB", [128, NE], F32)
    epsT = sb(es, "epsT", [128, 1], F32)
    r_const = Res()

    pbanks = [es.enter_context(nc.psum_tensor(f"pb{i}", [128, 512], F32)) for i in range(7)]
    pbT = pbanks[6] if cfg.get("NOPBT") else es.enter_context(nc.psum_tensor("pbT", [128, 512], F32))
    _pb = [Res(excl=True) for _ in range(8)]
    pres = [[_pb[i]] * 4 for i in range(7)]
    pTres = [_pb[7]] * 4

    sem_c = newsem("s_const")
    dsem = [newsem(f"s_d{i}") for i in range(20)]

    trk.dma(SP, sem_c, [(identb[:], c_identb[:, :]), (identf[:], c_identf[:, :]),
                        (dmask[:].rearrange("p h q -> p (h q)"), c_dmask[:, :]),
                        (qdecB[:].rearrange("p h q -> p (h q)"), c_qdec[:, :]),
                        (kdec[:], c_kdec[:, :]),
                        (rbB[:], router_b[0].partition_broadcast(128))], writes=[r_const])
    trk.op(DVE, lambda: nc.vector.memset(epsT[:], EPS), writes=[r_const])
    with contextlib.ExitStack() as st0:
        rwf = sb(st0, "rwf", [128, 16, NE], F32)
        r_rwf = Res()
        trk.dma(SP, sem_c, [(rwf[:], router_w.rearrange("(k p) e -> p k e", p=128))], writes=[r_rwf])
        trk.op(DVE, lambda: nc.vector.tensor_copy(rw16[:], rwf[:]), reads=[r_rwf], writes=[r_const])
        trk.barrier(ENGS)

    def tres(n):
        return [Res() for _ in range(n)]
    R_Xa, R_Xb, R_Ta, R_Tb, R_y = tres(T // 128), tres(T // 128), tres(T // 128), tres(T // 128), tres(T // 128)
    R_xin = tres(T // 128)
    R_w16s = [{k: Res() for k in ("in", "out", "gu", "d")} for _ in range(2)]

    cast_rr = [0]

    def cast_items(l):
        si = l % 2
        Win16, Wout16, Wgu16, Wd16, R_w16 = Win16s[si], Wout16s[si], Wgu16s[si], Wd16s[si], R_w16s[si]
        items = []
        wi = w_in[l * D:(l + 1) * D, :]
        for g in range(28):
            for hf in range(2):
                items.append(([(0, (8, 256), wi[hf * 1024:(hf + 1) * 1024, g * 256:(g + 1) * 256].rearrange("(k p) c -> p k c", p=128))],
                              2048, Win16[g * 128:(g + 1) * 128, hf * 2048:(hf + 1) * 2048], R_w16["in"]))
        wo = w_out[l * D:(l + 1) * D, :]
        for g in range(8):
            for hf in range(2):
                items.append(([(0, (8, 256), wo[hf * 1024:(hf + 1) * 1024, g * 256:(g + 1) * 256].rearrange("(k p) c -> p k c", p=128))],
                              2048, Wout16[g * 128:(g + 1) * 128, hf * 2048:(hf + 1) * 2048], R_w16["out"]))
        for e in range(NE):
            r0 = (l * NE + e) * D
            for ft in range(NFT):
                ti = e * NFT + ft
                for hf, wsrc in enumerate((w_gate, w_up)):
                    items.append(([(0, (16, 128), wsrc[r0:r0 + D, ft * 128:(ft + 1) * 128].rearrange("(k p) c -> p k c", p=128))],
                                  2048, Wgu16[ti * 128:(ti + 1) * 128, hf * 2048:(hf + 1) * 2048], R_w16["gu"]))
        nh = 2 if NFT >= 2 else 1
        fh = NFT // nh
        for e in range(NE):
            r0 = (l * NE + e) * DFF
            for dq in range(4):
                ti = e * 4 + dq
                for hf in range(nh):
                    items.append(([(0, (fh, 512), w_down[r0 + hf * fh * 128:r0 + (hf + 1) * fh * 128, dq * 512:(dq + 1) * 512].rearrange("(k p) c -> p k c", p=128))],
                                  fh * 512, Wd16[ti * 128:(ti + 1) * 128, hf * fh * 512:(hf + 1) * fh * 512], R_w16["d"]))
        return items

    class CastPump:
        def __init__(self, items, f32b, b16b, lsems, ssems, engines=None):
            self.items, self.f32b, self.b16b, self.lsems, self.ssems = items, f32b, b16b, lsems, ssems
            self.NB = len(f32b)
            self.rf = [Res() for _ in range(self.NB)]
            self.rb = [Res() for _ in range(self.NB)]
            self.engines = engines or ["pool"]
            self.i = 0

        def done(self):
            return self.i - 2 >= len(self.items)

        def pump(self):
            i, items = self.i, self.items
            if self.done():
                return
            NB = self.NB
            if 0 <= i - 1 < len(items):
                k = (i - 1) % NB
                n = items[i - 1][1]
                en = self.engines[(i - 1) % len(self.engines)]
                if en == "pool":
                    trk.op(POOL, lambda: nc.gpsimd.tensor_copy(self.b16b[k][:, 0:n], self.f32b[k][:, 0:n]), reads=[self.rf[k]], writes=[self.rb[k]])
                elif en == "act":
                    trk.op(ACT, lambda: nc.scalar.copy(self.b16b[k][:, 0:n], self.f32b[k][:, 0:n]), reads=[self.rf[k]], writes=[self.rb[k]])
                else:
                    trk.op(DVE, lambda: nc.vector.tensor_copy(self.b16b[k][:, 0:n], self.f32b[k][:, 0:n]), reads=[self.rf[k]], writes=[self.rb[k]])
            if 0 <= i - 2 < len(items):
                k = (i - 2) % NB
                _, n, dst, res = items[i - 2]
                trk.dma(SP, dsem[self.ssems[k]], [(dst, self.b16b[k][:, 0:n])], reads=[self.rb[k]], writes=[res])
            if i < len(items):
                k = i % NB
                srcs = items[i][0]
                pairs = [(self.f32b[k][:, off:off + a_ * b_].rearrange("p (a b) -> p a b", b=b_), src) for off, (a_, b_), src in srcs]
                trk.dma(SP, dsem[self.lsems[k]], pairs, writes=[self.rf[k]])
            self.i += 1

    def cast_layer_now(l):
        with contextlib.ExitStack() as st:
            f32b = [sb(st, f"cf{i}", [128, 2048], F32) for i in range(6)]
            b16b = [sb(st, f"cb{i}", [128, 2048], BF16) for i in range(6)]
            cp = CastPump(cast_items(l), f32b, b16b, [0, 1, 2, 3, 4, 5], [8, 9, 10, 11, 12, 13], engines=["pool", "act", "dve"])
            while not cp.done():
                cp.pump()
            trk.barrier(ENGS)

    def layer_norm_tile(ytile, ry, gB, bB, stats, mv, sc, rs):
        for c in range(4):
            trk.op(DVE, lambda c=c: nc.vector.bn_stats(stats[:, c, :], ytile[:, c * 512:(c + 1) * 512]), reads=[ry], writes=[rs])
        trk.op(DVE, lambda: nc.vector.bn_aggr(mv[:, 0:2], stats[:].rearrange("p c s -> p (c s)")), reads=[rs], writes=[rs])
        trk.op(ACT, lambda: nc.scalar.activation(sc[:, 0:1], mv[:, 1:2], AF.Sqrt, bias=epsT[:, 0:1], scale=1.0), reads=[rs, r_const], writes=[rs])
        trk.op(DVE, lambda: nc.vector.reciprocal(sc[:, 0:1], sc[:, 0:1]), reads=[rs], writes=[rs])
        trk.op(DVE, lambda: nc.vector.scalar_tensor_tensor(sc[:, 1:2], mv[:, 0:1], -1.0, sc[:, 0:1], ALU.mult, ALU.mult), reads=[rs], writes=[rs])
        trk.op(ACT, lambda: nc.scalar.activation(ytile[:], ytile[:], AF.Identity, bias=sc[:, 1:2], scale=sc[:, 0:1]), reads=[rs, ry], writes=[ry])
        trk.op(POOL, lambda: nc.gpsimd.tensor_tensor(ytile[:], ytile[:], gB[:], ALU.mult), reads=[ry, r_const], writes=[ry])
        trk.op(DVE, lambda: nc.vector.tensor_tensor(ytile[:], ytile[:], bB[:], ALU.add), reads=[ry, r_const], writes=[ry])

    tp_rr = [0]

    def transpose_tile_to_xT(src16, rsrc, dstT, col0, rdst):
        for g in range(8):
            half = tp_rr[0] % 2
            tp_rr[0] += 1
            for j in range(2):
                kc = g * 2 + j
                trk.op(PE, lambda kc=kc, j=j, half=half: nc.tensor.matmul(
                    pbT[:, half * 256 + j * 128: half * 256 + (j + 1) * 128], src16[:, kc * 128:(kc + 1) * 128], identb[:], start=True, stop=True),
                    reads=(rsrc if isinstance(rsrc, list) else [rsrc]) + [r_const], writes=[pTres[half * 2 + j]], signal=(j == 1))
            rr = [pTres[half * 2 + j] for j in range(2)]
            outv = dstT[:, g * 2:(g + 1) * 2, col0:col0 + 128]
            inv = pbT[:, half * 256:(half + 1) * 256].rearrange("p (j t) -> p j t", j=2)
            if g % 2 == 0:
                trk.op(ACT, lambda outv=outv, inv=inv: nc.scalar.copy(outv, inv), reads=rr, writes=[rdst])
            else:
                trk.op(DVE, lambda outv=outv, inv=inv: nc.vector.tensor_copy(outv, inv), reads=rr, writes=[rdst])

    def Tview(Tbuf, t0, n):
        return Tbuf[:, :, t0:t0 + n].rearrange("k p t -> p k t")

    def phase0():
        with contextlib.ExitStack() as st:
            xf = [sb(st, f"p0x{i}", [128, D], F32) for i in range(2)]
            xb = [sb(st, f"p0b{i}", [128, D], BF16) for i in range(2)]
            xT = [sb(st, f"p0t{i}", [128, 16, 128], BF16) for i in range(2)]
            rxf = [Res(), Res()]; rxb = [Res(), Res()]; rxT = [Res(), Res()]
            for t in range(T // 128):
                k = t % 2
                trk.dma(SP, dsem[k], [(xf[k][:], x_in[t * 128:(t + 1) * 128, :])], reads=[R_xin[t]], writes=[rxf[k]])
                P0 = cfg.get("P0", 9)
                if P0 >= 2:
                    trk.op(POOL, lambda k=k: nc.gpsimd.tensor_copy(xb[k][:], xf[k][:]), reads=[rxf[k]], writes=[rxb[k]])
                if P0 >= 3:
                    transpose_tile_to_xT(xb[k], rxb[k], xT[k], 0, rxT[k])
                if P0 >= 4:
                    trk.dma(SP, dsem[2 + k], [(Tview(Ta, t * 128, 128), xT[k][:])], reads=[rxT[k]], writes=[R_Ta[t]])
            trk.barrier(ENGS)

    def phaseA(l, Xsrc, R_Xsrc):
        with contextlib.ExitStack() as st:
            BLK = 256
            g1B = sb(st, "g1B", [128, D], F32); b1B = sb(st, "b1B", [128, D], F32)
            gainB = sb(st, "gainB", [128, 1024], F32)
            biasT = sb(st, "biasT", [128, 16, 640], BF16)
            trk.dma(SP, sem_c, [(g1B[:], ln1_g[l].partition_broadcast(128)), (b1B[:], ln1_b[l].partition_broadcast(128)),
                                (gainB[:], ret_gain[l].partition_broadcast(128))], writes=[r_const])
            with contextlib.ExitStack() as st2:
                mneg = sb(st2, "mneg", [128, 640], F32)
                bf = [sb(st2, f"bf{i}", [128, 640], F32) for i in range(2)]
                rbf = [Res(), Res()]
                trk.dma(SP, sem_c, [(mneg[:], c_maskneg[:, :])], writes=[r_const])
                for h in range(16):
                    k = h % 2
                    r0 = (l * 16 + h) * 128
                    trk.dma(SP, dsem[8 + k], [(bf[k][:], bias_exp[r0:r0 + 128, :])], writes=[rbf[k]])
                    trk.op(DVE, lambda h=h, k=k: nc.vector.tensor_tensor(biasT[:, h, :], bf[k][:], mneg[:], ALU.add),
                           reads=[rbf[k], r_const], writes=[r_const])
                trk.barrier(ENGS)

            xTb = [sb(st, f"xTb{i}", [128, 16, BLK], BF16) for i in range(2)]
            rxTb = [Res(), Res()]
            x32 = [sb(st, f"x32_{i}", [128, D], F32) for i in range(2)]
            rx32 = [Res(), Res()]
            cs = [sb(st, f"cs{i}", [128, 2, BLK], F32) for i in range(2)]
            rcs = [Res(), Res()]
            NW = 3
            wp = [sb(st, f"wp{i}", [128, 16, 256], BF16) for i in range(NW)]
            rwp = [Res() for _ in range(NW)]
            qkT = sb(st, "qkT", [128, 16, BLK], BF16)
            rqk = [Res() for _ in range(8)]
            qTa = sb(st, "qTa", [128, 8, BLK], BF16)
            rqa = [Res() for _ in range(4)]
            RING = 6
            Kring = sb(st, "Kring", [128, 8, RING * 128], BF16)
            Vring = sb(st, "Vring", [128, RING, 16, 65], BF16)
            rK = [[Res() for _ in range(4)] for _ in range(RING)]
            rV = [[Res() for _ in range(4)] for _ in range(RING)]
            vret = sb(st, "vret", [128, 2, 1024], BF16)
            rvret = [[Res() for _ in range(4)] for _ in range(2)]
            sg = sb(st, "sg", [128, 2, 1024], BF16)
            rsg = [[Res() for _ in range(4)] for _ in range(2)]
            mixed = sb(st, "mixed", [128, D], BF16)
            rmixed_r = Res(); rmixed_a = Res()
            ahst = [sb(st, f"ahst{i}", [128, 2], F32) for i in range(2)]
            rahst = [Res(), Res()]
            acnt = [0]
            NEB = 12
            mixedT = sb(st, "mixedT", [128, 16, BLK], BF16)
            rmT = [Res(), Res()]
            state = sb(st, "state", [128, 4, 2, 256], F32)
            state16 = sb(st, "state16", [128, 4, 2, 256], BF16)
            rstate = [Res() for _ in range(4)]
            t1s = sb(st, "t1s", [128, BLK], F32); t2s = sb(st, "t2s", [128, BLK], F32)
            ra = sb(st, "ra", [128, BLK], F32); rbt = sb(st, "rbt", [128, BLK], F32)
            rc = sb(st, "rc", [128, BLK], F32); rd = sb(st, "rd", [128, BLK], F32)
            rrot = Res(); rrot2 = Res()
            PTs = [sb(st, f"PT{i}", [128, 128], BF16) for i in range(2)]
            rPT = [Res(), Res()]
            Es = [sb(st, f"E{i}", [128, 128], BF16) for i in range(12)]
            rE = [Res() for _ in range(12)]
            qd = [sb(st, f"qd{i}", [128, 2, 128], BF16) for i in range(2)]
            rqd = [Res(), Res()]
            ktd = [sb(st, f"ktd{i}", [128, 256], BF16) for i in range(2)]
            rktd = [Res(), Res()]
            onb = [sb(st, f"on{i}", [128, 256], F32) for i in range(2)]
            ron = [Res(), Res()]
            stats = sb(st, "stats", [128, 4, 6], F32); mv = sb(st, "mv", [128, 4], F32); sc = sb(st, "sc", [128, 4], F32)
            rs = Res()
            hst = [sb(st, f"hst{i}", [128, 8], F32) for i in range(2)]
            rhst = [Res(), Res()]
            xn16 = sb(st, "xn16", [128, D], BF16); rxn = Res()
            xTs = sb(st, "xTs", [128, 16, BLK], BF16); rxTs = Res()

            trk.op(POOL, lambda: nc.gpsimd.memset(Vring[:].rearrange("p r h e -> p (r h e)"), 1.0), writes=[rV[i][j] for i in range(RING) for j in range(4)])

            wcnt = [0]
            ecnt = [0]
            pcnt = {"acc": 0, "sc": 0, "o": 0, "kv": 0, "ao": 0}

            def load_w(src, r0, c0):
                k = wcnt[0] % NW
                wcnt[0] += 1
                g_ = c0 // 256
                trk.dma(SP, dsem[6 + k], [(wp[k][:].rearrange("p k c -> p (k c)"), src[g_ * 128:(g_ + 1) * 128, :])],
                        reads=[R_w16s[l % 2]["in"], R_w16s[l % 2]["out"]], writes=[rwp[k]])
                return wp[k], rwp[k]

            nblk = S // BLK
            for s in range(NSEQ):
                for h in range(4):
                    trk.op(POOL, lambda h=h: nc.gpsimd.memset(state[:, h].rearrange("p a b -> p (a b)"), 0.0), writes=[rstate[h]])
                    trk.op(POOL, lambda h=h: nc.gpsimd.memset(state16[:, h].rearrange("p a b -> p (a b)"), 0.0), writes=[rstate[h]])
                for b in range(nblk):
                    tok0 = s * S + b * BLK
                    gt0 = tok0 // 128
                    kb = b % 2
                    trk.dma(SP, dsem[kb], [(xTb[kb][:], Tview(Ta, tok0, BLK))], reads=[R_Ta[gt0], R_Ta[gt0 + 1]], writes=[rxTb[kb]])
                    trk.dma(SP, dsem[2 + kb], [(cs[kb][:, 0, :], c_cos[:, b * BLK:(b + 1) * BLK]), (cs[kb][:, 1, :], c_sin[:, b * BLK:(b + 1) * BLK])], writes=[rcs[kb]])
                    xt = xTb[kb]
                    for g in range(28):
                        wt, rw = load_w(Win16s[l % 2], 0, g * 256)
                        kind = g // 4
                        if kind in (0, 1, 4, 5):
                            bank = pcnt["acc"] % 2
                            pcnt["acc"] += 1
                            for j in range(2):
                                for kc in range(16):
                                    trk.op(PE, lambda j=j, kc=kc: nc.tensor.matmul(pbanks[bank][:, j * 256:(j + 1) * 256], wt[:, kc, j * 128:(j + 1) * 128], xt[:, kc, :],
                                                                                  start=(kc == 0), stop=(kc == 15)),
                                           reads=[rw, rxTb[kb]], writes=[pres[bank][2 * j], pres[bank][2 * j + 1]], signal=(kc == 15))
                            pr = pres[bank]
                            if kind in (0, 1):
                                hh = g % 4
                                scl = 1.0 if kind == 0 else 1.0 / 16.0
                                trk.op(ACT, lambda: nc.scalar.activation(t1s[:], pbanks[bank][:, 0:256], AF.Copy, scale=scl), reads=[pr[0], pr[1]], writes=[rrot])
                                trk.op(ACT, lambda: nc.scalar.activation(t2s[:], pbanks[bank][:, 256:512], AF.Copy, scale=scl), reads=[pr[2], pr[3]], writes=[rrot])
                                cosv = cs[kb][:, 0, :]; sinv = cs[kb][:, 1, :]
                                o1 = qkT[:, kind * 8 + 2 * hh, :]; o2 = qkT[:, kind * 8 + 2 * hh + 1, :]
                                rq = rqk[kind * 4 + hh]
                                trk.op(DVE, lambda: nc.vector.tensor_tensor(ra[:], t1s[:], cosv, ALU.mult), reads=[rrot, rcs[kb]], writes=[rrot2])
                                trk.op(POOL, lambda: nc.gpsimd.tensor_tensor(rbt[:], t2s[:], sinv, ALU.mult), reads=[rrot, rcs[kb]], writes=[rrot2])
                                trk.op(DVE, lambda: nc.vector.tensor_tensor(rc[:], t1s[:], sinv, ALU.mult), reads=[rrot, rcs[kb]], writes=[rrot2])
                                trk.op(POOL, lambda: nc.gpsimd.tensor_tensor(rd[:], t2s[:], cosv, ALU.mult), reads=[rrot, rcs[kb]], writes=[rrot2])
                                trk.op(DVE, lambda: nc.vector.tensor_tensor(o1, ra[:], rbt[:], ALU.subtract), reads=[rrot2], writes=[rq])
                                trk.op(POOL, lambda: nc.gpsimd.tensor_tensor(o2, rc[:], rd[:], ALU.add), reads=[rrot2], writes=[rq])
                            elif kind == 4:
                                gg = g % 4
                                trk.op(ACT, lambda: nc.scalar.activation(qTa[:, 2 * gg:2 * gg + 2, :], pbanks[bank][:].rearrange("p (j t) -> p j t", j=2), AF.Copy, scale=0.125),
                                       reads=pr, writes=[rqa[gg]])
                            else:
                                gg = g % 4
                                for tt in range(2):
                                    slot = (2 * b + tt) % RING
                                    trk.op(DVE, lambda tt=tt, slot=slot: nc.vector.tensor_copy(
                                        Kring[:, 2 * gg:2 * gg + 2, slot * 128:(slot + 1) * 128],
                                        pbanks[bank][:].rearrange("p (j t) -> p j t", j=2)[:, :, tt * 128:(tt + 1) * 128]),
                                        reads=pr, writes=[rK[slot][gg]])
                        else:
                            gg = g % 4
                            for tt in range(2):
                                bank = pcnt["acc"] % 2
                                pcnt["acc"] += 1
                                for kc in range(16):
                                    trk.op(PE, lambda kc=kc, tt=tt: nc.tensor.matmul(pbanks[bank][:, 0:256], xt[:, kc, tt * 128:(tt + 1) * 128], wt[:, kc, :],
                                                                                  start=(kc == 0), stop=(kc == 15)),
                                           reads=[rw, rxTb[kb]], writes=[pres[bank][0], pres[bank][1]], signal=(kc == 15))
                                pr = [pres[bank][0], pres[bank][1]]
                                if kind == 2:
                                    trk.op(ACT, lambda tt=tt: nc.scalar.copy(vret[:, tt, gg * 256:(gg + 1) * 256], pbanks[bank][:, 0:256]), reads=pr, writes=[rvret[tt][gg]])
                                elif kind == 3:
                                    trk.op(ACT, lambda tt=tt: nc.scalar.activation(onb[tt][:], pbanks[bank][:, 0:256], AF.Silu), reads=pr, writes=[ron[tt]])
                                    trk.op(POOL, lambda tt=tt: nc.gpsimd.tensor_tensor(sg[:, tt, gg * 256:(gg + 1) * 256], onb[tt][:], gainB[:, gg * 256:(gg + 1) * 256], ALU.mult),
                                           reads=[ron[tt], r_const], writes=[rsg[tt][gg]])
                                else:
                                    slot = (2 * b + tt) % RING
                                    trk.op(DVE, lambda tt=tt, slot=slot: nc.vector.tensor_copy(
                                        Vring[:, slot, 4 * gg:4 * gg + 4, 0:64], pbanks[bank][:, 0:256].rearrange("p (h e) -> p h e", h=4)),
                                        reads=pr, writes=[rV[slot][gg]])
                    for tt in range(2):
                        tis = 2 * b + tt
                        slot = tis % RING
                        cols = slice(tt * 128, (tt + 1) * 128)
                        def ret_stage1(h):
                            kq = pcnt["sc"] % 8; pcnt["sc"] += 1
                            sbank, sreg = 2 + kq % 2, kq // 2
                            sps = pbanks[sbank][:, sreg * 128:(sreg + 1) * 128]
                            rsp = pres[sbank][sreg]
                            for dc in range(2):
                                trk.op(PE, lambda dc=dc: nc.tensor.matmul(sps, qkT[:, 8 + 2 * h + dc, cols], qkT[:, 2 * h + dc, cols], start=(dc == 0), stop=(dc == 1)),
                                       reads=[rqk[4 + h], rqk[h]], writes=[rsp], signal=(dc == 1))
                            kp = ecnt[0] % 2; ecnt[0] += 1
                            trk.op(DVE, lambda: nc.vector.tensor_tensor(PTs[kp][:], sps, dmask[:, h, :], ALU.mult), reads=[rsp, r_const], writes=[rPT[kp]])
                            for dc in range(2):
                                trk.op(POOL, lambda dc=dc: nc.gpsimd.tensor_tensor(qd[kp][:, dc, :], qkT[:, 2 * h + dc, cols], qdecB[:, h, :], ALU.mult),
                                       reads=[rqk[h], r_const], writes=[rqd[kp]])
                            half = tp_rr[0] % 2; tp_rr[0] += 1
                            for dc in range(2):
                                trk.op(PE, lambda dc=dc: nc.tensor.matmul(pbT[:, half * 256 + dc * 128: half * 256 + (dc + 1) * 128], qkT[:, 8 + 2 * h + dc, cols], identb[:], start=True, stop=True),
                                       reads=[rqk[4 + h], r_const], writes=[pTres[half * 2 + dc]], signal=(dc == 1))
                            trk.op(ACT, lambda: nc.scalar.activation(ktd[kp][:], pbT[:, half * 256: half * 256 + 256], AF.Identity, scale=kdec[:, h:h + 1]),
                                   reads=[pTres[half * 2], pTres[half * 2 + 1], r_const], writes=[rktd[kp]])
                            return kp

                        def ret_stage2(h, kp):
                            ko = pcnt["o"] % 2; pcnt["o"] += 1
                            ops = pbanks[4][:, ko * 256:(ko + 1) * 256]
                            rop = [pres[4][2 * ko], pres[4][2 * ko + 1]]
                            vv = vret[:, tt, h * 256:(h + 1) * 256]
                            trk.op(PE, lambda: nc.tensor.matmul(ops, PTs[kp][:], vv, start=True, stop=False), reads=[rPT[kp], rvret[tt][h]], writes=rop, signal=False)
                            for dc in range(2):
                                trk.op(PE, lambda dc=dc: nc.tensor.matmul(ops, qd[kp][:, dc, :], state16[:, h, dc, :], start=False, stop=(dc == 1)),
                                       reads=[rqd[kp], rstate[h]], writes=rop, signal=(dc == 1))
                            kvp = []
                            for dc in range(2):
                                kk = pcnt["kv"] % 2; pcnt["kv"] += 1
                                kps = pbanks[5][:, kk * 256:(kk + 1) * 256]
                                rkp = [pres[5][2 * kk], pres[5][2 * kk + 1]]
                                trk.op(PE, lambda dc=dc: nc.tensor.matmul(kps, ktd[kp][:, dc * 128:(dc + 1) * 128], vv, start=True, stop=True), reads=[rktd[kp], rvret[tt][h]], writes=rkp)
                                kvp.append((kps, rkp))
                            kh = kp
                            trk.op(DVE, lambda: nc.vector.bn_stats(hst[kh][:, 0:6], ops), reads=rop, writes=[rhst[kh]])
                            trk.op(DVE, lambda: nc.vector.bn_aggr(hst[kh][:, 6:8], hst[kh][:, 0:6]), reads=[rhst[kh]], writes=[rhst[kh]])
                            trk.op(ACT, lambda: nc.scalar.activation(hst[kh][:, 0:1], hst[kh][:, 7:8], AF.Sqrt, bias=epsT[:, 0:1], scale=1.0), reads=[rhst[kh], r_const], writes=[rhst[kh]])
                            trk.op(DVE, lambda: nc.vector.reciprocal(hst[kh][:, 0:1], hst[kh][:, 0:1]), reads=[rhst[kh]], writes=[rhst[kh]])
                            trk.op(DVE, lambda: nc.vector.scalar_tensor_tensor(hst[kh][:, 1:2], hst[kh][:, 6:7], -1.0, hst[kh][:, 0:1], ALU.mult, ALU.mult), reads=[rhst[kh]], writes=[rhst[kh]])
                            trk.op(ACT, lambda: nc.scalar.activation(onb[kh][:], ops, AF.Identity, bias=hst[kh][:, 1:2], scale=hst[kh][:, 0:1]), reads=rop + [rhst[kh]], writes=[ron[kh]])
                            trk.op(POOL, lambda: nc.gpsimd.tensor_tensor(mixed[:, h * 256:(h + 1) * 256], onb[kh][:], sg[:, tt, h * 256:(h + 1) * 256], ALU.mult),
                                   reads=[ron[kh], rsg[tt][h]], writes=[rmixed_r])
                            g128 = float(np.exp(128.0 * np.log1p(-2.0 ** (-5.0 - h))))
                            for dc in range(2):
                                kps, rkp = kvp[dc]
                                trk.op(DVE, lambda dc=dc, kps=kps: nc.vector.scalar_tensor_tensor(state[:, h, dc, :], state[:, h, dc, :], g128, kps, ALU.mult, ALU.add), reads=rkp + [rstate[h]], writes=[rstate[h]])
                                trk.op(POOL, lambda dc=dc: nc.gpsimd.tensor_copy(state16[:, h, dc, :], state[:, h, dc, :]), reads=[rstate[h]], writes=[rstate[h]])

                        kts = [kt for kt in range(5) if tis - 4 + kt >= 0]

                        def att_ST(h):
                            hp, pb0 = h // 2, 64 * (h % 2)
                            ecur = []
                            for kt in kts:
                                kslot = (tis - 4 + kt) % RING
                                kq = pcnt["sc"] % 8; pcnt["sc"] += 1
                                sbank, sreg = 2 + kq % 2, kq // 2
                                sps = pbanks[sbank][:, sreg * 128:(sreg + 1) * 128]
                                rsp = pres[sbank][sreg]
                                trk.op(PE, lambda: nc.tensor.matmul(sps, Kring[pb0:pb0 + 64, hp, kslot * 128:(kslot + 1) * 128], qTa[pb0:pb0 + 64, hp, cols], start=True, stop=False),
                                       reads=[rK[kslot][hp // 2], rqa[hp // 2]], writes=[rsp], signal=False)
                                trk.op(PE, lambda: nc.tensor.matmul(sps, identb[:], biasT[:, h, kt * 128:(kt + 1) * 128], start=False, stop=True), reads=[r_const], writes=[rsp])
                                ke = acnt[0] % NEB; acnt[0] += 1
                                trk.op(ACT, lambda: nc.scalar.activation(Es[ke][:], sps, AF.Exp), reads=[rsp], writes=[rE[ke]])
                                ecur.append((ke, kslot))
                            return (h, ecur)

                        def att_PV(ctx):
                            h, ecur = ctx
                            ka = pcnt["ao"] % 4; pcnt["ao"] += 1
                            aps = pbanks[6][:, ka * 128: ka * 128 + 65]
                            rap = pres[6][ka]
                            for i, (ke, kslot) in enumerate(ecur):
                                trk.op(PE, lambda: nc.tensor.matmul(aps, Es[ke][:], Vring[:, kslot, h, :], start=(i == 0), stop=(i == len(ecur) - 1)),
                                       reads=[rE[ke], rV[kslot][h // 4]], writes=[rap], signal=(i == len(ecur) - 1))
                            kh = ka % 2
                            trk.op(DVE, lambda: nc.vector.reciprocal(ahst[kh][:, 0:1], pbanks[6][:, ka * 128 + 64: ka * 128 + 65]), reads=[rap], writes=[rahst[kh]])
                            trk.op(DVE, lambda: nc.vector.tensor_scalar(mixed[:, 1024 + h * 64: 1024 + (h + 1) * 64], pbanks[6][:, ka * 128: ka * 128 + 64], ahst[kh][:, 0:1], None, ALU.mult),
                                   reads=[rap, rahst[kh]], writes=[rmixed_a])

                        prev_att = [None]

                        def att(h):
                            cur = att_ST(h)
                            if prev_att[0] is not None:
                                att_PV(prev_att[0])
                            prev_att[0] = cur

                        for h in range(4):
                            kp_ = ret_stage1(h)
                            att(4 * h); att(4 * h + 1)
                            ret_stage2(h, kp_)
                            att(4 * h + 2); att(4 * h + 3)
                        att_PV(prev_att[0])
                        transpose_tile_to_xT(mixed, [rmixed_r, rmixed_a], mixedT, tt * 128, rmT[tt])
                    wts = [load_w(Wout16s[l % 2], 0, g * 256) for g in range(3)]
                    for tt in range(2):
                        gt = gt0 + tt
                        kx = tt
                        trk.dma(SP, dsem[4 + kx], [(x32[kx][:], Xsrc[gt * 128:(gt + 1) * 128, :])], reads=[R_Xsrc[gt]], writes=[rx32[kx]])
                    for g in range(8):
                        if g < 3:
                            wt, rw = wts[g]
                        else:
                            wt, rw = load_w(Wout16s[l % 2], 0, g * 256)
                        for tt in range(2):
                            bank = pcnt["acc"] % 2; pcnt["acc"] += 1
                            for mc in range(16):
                                trk.op(PE, lambda mc=mc, tt=tt: nc.tensor.matmul(pbanks[bank][:, 0:256], mixedT[:, mc, tt * 128:(tt + 1) * 128], wt[:, mc, :], start=(mc == 0), stop=(mc == 15)),
                                       reads=[rw, rmT[tt]], writes=[pres[bank][0], pres[bank][1]], signal=(mc == 15))
                            yv = x32[tt][:, g * 256:(g + 1) * 256]
                            trk.op(DVE, lambda yv=yv: nc.vector.scalar_tensor_tensor(yv, yv, ALPHA, pbanks[bank][:, 0:256], ALU.mult, ALU.add),
                                   reads=[pres[bank][0], pres[bank][1], rx32[tt]], writes=[rx32[tt]])
                    for tt in range(2):
                        gt = gt0 + tt
                        layer_norm_tile(x32[tt], rx32[tt], g1B, b1B, stats, mv, sc, rs)
                        trk.dma(SP, dsem[10 + tt], [(Xb[gt * 128:(gt + 1) * 128, :], x32[tt][:])], reads=[rx32[tt]], writes=[R_Xb[gt]])
                        trk.op(ACT, lambda tt=tt: nc.scalar.copy(xn16[:], x32[tt][:]), reads=[rx32[tt]], writes=[rxn])
                        transpose_tile_to_xT(xn16, rxn, xTs, tt * 128, rxTs)
                    trk.dma(SP, dsem[12], [(Tview(Tb, tok0, BLK), xTs[:])], reads=[rxTs], writes=[R_Tb[gt0], R_Tb[gt0 + 1]])
            trk.barrier(ENGS)

    def phaseB(l, Xdst, R_Xdst, last):
        with contextlib.ExitStack() as st:
            BLK = 512
            Wgu16, Wd16, R_w16 = Wgu16s[l % 2], Wd16s[l % 2], R_w16s[l % 2]
            cp = None
            if not last and cfg.get("OVERLAP_CAST", 1):
                cf32 = [sb(st, f"pcf{i}", [128, 2048], F32) for i in range(2)]
                cb16 = [sb(st, f"pcb{i}", [128, 2048], BF16) for i in range(2)]
                cp = CastPump(cast_items(l + 1), cf32, cb16, [16, 17], [18, 19])
            g2B = sb(st, "g2B", [128, D], F32); b2B = sb(st, "b2B", [128, D], F32)
            selT = sb(st, "selT", [16, 16, 128], F32)
            trk.dma(SP, sem_c, [(g2B[:], ln2_g[l].partition_broadcast(128)), (b2B[:], ln2_b[l].partition_broadcast(128)),
                                (selT[:].rearrange("p e q -> p (e q)"), c_sel[:, :])], writes=[r_const])
            xTb = [sb(st, f"bxT{i}", [128, 16, BLK], BF16) for i in range(2)]
            rxTb = [Res(), Res()]
            acc = [sb(st, f"acc{i}", [128, D], F32) for i in range(4)]
            racc = [Res() for _ in range(4)]
            NG = 2
            gu = [sb(st, f"gu{i}", [128, 2, 16, 128], BF16) for i in range(NG)]
            rgu = [Res() for _ in range(NG)]
            ND = 2
            dw = [sb(st, f"dw{i}", [128, NFT, 512], BF16) for i in range(ND)]
            rdw = [Res() for _ in range(ND)]
            hT = [sb(st, f"hT{i}", [128, NFT, BLK], BF16) for i in range(2)]
            rhT = [Res(), Res()]
            cbB = sb(st, "cbB", [128, NE, BLK], BF16); rcbB = Res()
            cbT = sb(st, "cbT", [16, BLK], F32); rcbT = Res()
            sil = [sb(st, f"sil{i}", [128, BLK], F32) for i in range(2)]
            rsil = [Res(), Res()]
            sa = [sb(st, f"sa{i}", [128, BLK], F32) for i in range(2)]
            rsa = [Res(), Res()]
            rt = sb(st, "rt", [128, 160], F32); rrt = Res()
            stats = sb(st, "bstats", [128, 4, 6], F32); mv = sb(st, "bmv", [128, 4], F32); sc = sb(st, "bsc", [128, 4], F32)
            rs = Res()
            xn16 = sb(st, "bxn16", [128, D], BF16); rxn = Res()
            xTs = sb(st, "bxTs", [128, 16, 256], BF16); rxTs = Res()
            gcnt = [0]; dcnt = [0]; cnt = {"g": 0, "d": 0, "s": 0}

            for bi in range(T // BLK):
                tok0 = bi * BLK
                gt0 = tok0 // 128
                kb = bi % 2
                trk.dma(SP, dsem[kb], [(xTb[kb][:], Tview(Tb, tok0, BLK))], reads=[R_Tb[gt0 + i] for i in range(4)], writes=[rxTb[kb]])
                xt = xTb[kb]
                for tt in range(4):
                    trk.dma(SP, dsem[2 + tt], [(acc[tt][:], Xb[(gt0 + tt) * 128:(gt0 + tt + 1) * 128, :])], reads=[R_Xb[gt0 + tt]], writes=[racc[tt]])
                for tt in range(4):
                    lg = pbanks[6][:, 0:16]
                    for kc in range(16):
                        trk.op(PE, lambda kc=kc: nc.tensor.matmul(lg, xt[:, kc, tt * 128:(tt + 1) * 128], rw16[:, kc, :], start=(kc == 0), stop=(kc == 15)),
                               reads=[rxTb[kb], r_const], writes=[pres[6][0]], signal=(kc == 15))
                    aff = rt[:, 0:16]; sel = rt[:, 16:32]; ps6 = rt[:, 32:56]; grp = rt[:, 56:60]; gmx = rt[:, 60:61]
                    eqg = rt[:, 61:65]; msk = rt[:, 65:81]; t1 = rt[:, 81:82]; eq1 = rt[:, 82:98]; m2 = rt[:, 98:114]
                    t2 = rt[:, 114:115]; eq2 = rt[:, 115:131]; wsel = rt[:, 131:147]; den = rt[:, 147:148]; cb = rt[:, 148:164] if False else None
                    V = nc.vector
                    R = dict(reads=[rrt], writes=[rrt])
                    trk.op(ACT, lambda: nc.scalar.activation(aff, lg, AF.Sigmoid), reads=[pres[6][0]], writes=[rrt])
                    trk.op(DVE, lambda: V.tensor_tensor(sel, aff, rbB[:], ALU.add), reads=[rrt, r_const], writes=[rrt])
                    sel3 = sel.rearrange("p (g e) -> p g e", e=4)
                    ps3 = ps6.rearrange("p (g c) -> p g c", c=6)
                    for idx, (i, j) in enumerate([(0, 1), (0, 2), (0, 3), (1, 2), (1, 3), (2, 3)]):
                        trk.op(DVE, lambda idx=idx, i=i, j=j: V.tensor_tensor(ps3[:, :, idx], sel3[:, :, i], sel3[:, :, j], ALU.add), **R)
                    trk.op(DVE, lambda: V.tensor_reduce(grp, ps3, AX.X, ALU.max), **R)
                    trk.op(DVE, lambda: V.tensor_reduce(gmx, grp, AX.X, ALU.max), **R)
                    trk.op(DVE, lambda: V.tensor_scalar(eqg, grp, gmx, None, ALU.is_equal), **R)
                    msk3 = msk.rearrange("p (g e) -> p g e", e=4)
                    for e in range(4):
                        trk.op(DVE, lambda e=e: V.scalar_tensor_tensor(msk3[:, :, e], sel3[:, :, e], 10.0, eqg, ALU.add, ALU.mult), **R)
                    trk.op(DVE, lambda: V.tensor_reduce(t1, msk, AX.X, ALU.max), **R)
                    trk.op(DVE, lambda: V.tensor_scalar(eq1, msk, t1, None, ALU.is_equal), **R)
                    trk.op(DVE, lambda: V.scalar_tensor_tensor(m2, eq1, -100.0, msk, ALU.mult, ALU.add), **R)
                    trk.op(DVE, lambda: V.tensor_reduce(t2, m2, AX.X, ALU.max), **R)
                    trk.op(DVE, lambda: V.tensor_scalar(eq2, m2, t2, None, ALU.is_equal), **R)
                    trk.op(DVE, lambda: V.tensor_tensor(eq1, eq1, eq2, ALU.add), **R)
                    trk.op(DVE, lambda: V.tensor_tensor(wsel, aff, eq1, ALU.mult), **R)
                    trk.op(DVE, lambda: V.tensor_reduce(den, wsel, AX.X, ALU.add), **R)
                    trk.op(DVE, lambda: V.reciprocal(den, den), **R)
                    trk.op(DVE, lambda: V.tensor_scalar(wsel, wsel, den, None, ALU.mult), **R)
                    trk.op(PE, lambda: nc.tensor.matmul(pbanks[6][0:16, 128:256], wsel, identf[:], start=True, stop=True), reads=[rrt, r_const], writes=[pres[6][1]])
                    trk.op(ACT, lambda tt=tt: nc.scalar.copy(cbT[:, tt * 128:(tt + 1) * 128], pbanks[6][0:16, 128:256]), reads=[pres[6][1]], writes=[rcbT])
                for e in range(NE):
                    trk.op(PE, lambda e=e: nc.tensor.matmul(pbanks[6][:, :], selT[:, e, :], cbT[:, :], start=True, stop=True), reads=[rcbT, r_const], writes=pres[6])
                    trk.op(ACT, lambda e=e: nc.scalar.copy(cbB[:, e, :], pbanks[6][:, :]), reads=pres[6], writes=[rcbB])
                for e in range(NE):
                    kh = e % 2
                    for ft in range(NFT):
                        if cp is not None:
                            cp.pump()
                        k = gcnt[0] % NG; gcnt[0] += 1
                        ti = e * NFT + ft
                        trk.dma(SP, dsem[6 + k], [(gu[k][:].rearrange("p a k c -> p (a k c)"), Wgu16[ti * 128:(ti + 1) * 128, :])],
                                reads=[R_w16["gu"]], writes=[rgu[k]])
                        gb = cnt["g"] % 2; cnt["g"] += 1
                        for which in range(2):
                            bank = gb * 2 + which
                            for kc in range(16):
                                trk.op(PE, lambda kc=kc, which=which, bank=bank: nc.tensor.matmul(pbanks[bank][:, :], gu[k][:, which, kc, :], xt[:, kc, :], start=(kc == 0), stop=(kc == 15)),
                                       reads=[rgu[k], rxTb[kb]], writes=pres[bank], signal=(kc == 15))
                        ks = cnt["s"] % 2; cnt["s"] += 1
                        trk.op(ACT, lambda: nc.scalar.activation(sil[ks][:], pbanks[gb * 2][:, :], AF.Silu), reads=pres[gb * 2], writes=[rsil[ks]])
                        trk.op(POOL, lambda: nc.gpsimd.tensor_tensor(sa[ks][:], sil[ks][:], cbB[:, e, :], ALU.mult), reads=[rsil[ks], rcbB], writes=[rsa[ks]])
                        trk.op(DVE, lambda: nc.vector.tensor_tensor(hT[kh][:, ft, :], sa[ks][:], pbanks[gb * 2 + 1][:, :], ALU.mult), reads=[rsa[ks]] + pres[gb * 2 + 1], writes=[rhT[kh]])
                    for dq in range(4):
                        k = dcnt[0] % ND; dcnt[0] += 1
                        ti = e * 4 + dq
                        trk.dma(SP, dsem[8 + k], [(dw[k][:].rearrange("p k c -> p (k c)"), Wd16[ti * 128:(ti + 1) * 128, :])],
                                reads=[R_w16["d"]], writes=[rdw[k]])
                        for tt in range(4):
                            bank = 4 + cnt["d"] % 2; cnt["d"] += 1
                            for ft in range(NFT):
                                trk.op(PE, lambda ft=ft, tt=tt, bank=bank: nc.tensor.matmul(pbanks[bank][:, :], hT[kh][:, ft, tt * 128:(tt + 1) * 128], dw[k][:, ft, :], start=(ft == 0), stop=(ft == NFT - 1)),
                                       reads=[rhT[kh], rdw[k]], writes=pres[bank], signal=(ft == NFT - 1))
                            av = acc[tt][:, dq * 512:(dq + 1) * 512]
                            if e == 0:
                                trk.op(DVE, lambda av=av, bank=bank: nc.vector.scalar_tensor_tensor(av, av, ALPHA, pbanks[bank][:, :], ALU.mult, ALU.add), reads=pres[bank] + [racc[tt]], writes=[racc[tt]])
                            else:
                                trk.op(DVE, lambda av=av, bank=bank: nc.vector.tensor_tensor(av, av, pbanks[bank][:, :], ALU.add), reads=pres[bank] + [racc[tt]], writes=[racc[tt]])
                for tt in range(4):
                    gt = gt0 + tt
                    layer_norm_tile(acc[tt], racc[tt], g2B, b2B, stats, mv, sc, rs)
                    trk.dma(SP, dsem[10 + tt], [(Xdst[gt * 128:(gt + 1) * 128, :], acc[tt][:])], reads=[racc[tt]], writes=[R_Xdst[gt]])
                    if not last:
                        trk.op(ACT, lambda tt=tt: nc.scalar.copy(xn16[:], acc[tt][:]), reads=[racc[tt]], writes=[rxn])
                        transpose_tile_to_xT(xn16, rxn, xTs, (tt % 2) * 128, rxTs)
                        if tt % 2 == 1:
                            trk.dma(SP, dsem[14], [(Tview(Ta, tok0 + (tt - 1) * 128, 256), xTs[:])], reads=[rxTs], writes=[R_Ta[gt - 1], R_Ta[gt]])
            if cp is not None:
                while not cp.done():
                    cp.pump()
            trk.barrier(ENGS)

    STOP = cfg.get("STOP", 99)
    if STOP >= 0:
        phase0()
    for l in range(L):
        if STOP <= 0:
            break
        if l == 0 or not cfg.get("OVERLAP_CAST", 1):
            cast_layer_now(l)
        if STOP <= 1:
            break
        if l == 0:
            phaseA(l, x_in, R_xin)
        else:
            phaseA(l, Xa, R_Xa)
        last = (l == L - 1)
        if STOP <= 2:
            break
        phaseB(l, y_out if last else Xa, R_y if last else R_Xa, last)
    trk.barrier(ENGS)
    es.close()
    return nc


def host_constants(cfg):
    S = cfg["S"]
    half = 128
    inv = (10000.0 ** (-np.arange(half, dtype=np.float32) / half)).astype(np.float32)
    ang = (np.arange(S, dtype=np.float32)[None, :] * inv[:, None]).astype(np.float32)
    c = {}
    c["c_cos"] = np.cos(ang).astype(np.float32)
    c["c_sin"] = np.sin(ang).astype(np.float32)
    lg = np.log1p(-np.exp2(-5.0 - np.arange(4, dtype=np.float32))).astype(np.float32)
    k = np.arange(128)[:, None]; q = np.arange(128)[None, :]
    dm = np.zeros((128, 4, 128), np.float32)
    for h in range(4):
        m = np.exp(lg[h] * np.abs(q - k).astype(np.float32)).astype(np.float32)
        m = np.where((k // 64) <= (q // 64), m, 0.0)
        dm[:, h, :] = m
    c["c_dmask"] = dm.reshape(128, 512)
    qd = np.zeros((128, 4, 128), np.float32)
    kd = np.zeros((128, 4), np.float32)
    for h in range(4):
        qd[:, h, :] = np.exp(lg[h] * (np.arange(128, dtype=np.float32) + 1.0))[None, :]
        kd[:, h] = np.exp(lg[h] * (127.0 - np.arange(128, dtype=np.float32)))
    c["c_qdec"] = qd.reshape(128, 512)
    c["c_kdec"] = kd
    c["c_identb"] = np.eye(128, dtype=np.float32).astype(ml_dtypes.bfloat16)
    c["c_identf"] = np.eye(128, dtype=np.float32)
    mneg = np.zeros((128, 5, 128), np.float32)
    mneg[64:128, 4, 0:64] = -30000.0
    mneg[0:64, 0, 64:128] = -30000.0
    c["c_maskneg"] = mneg.reshape(128, 640)
    sel = np.zeros((16, 16, 128), np.float32)
    for e in range(16):
        sel[e, e, :] = 1.0
    c["c_sel"] = sel.reshape(16, 2048)
    return c


def bias_index():
    k = np.arange(128)[:, None, None]; kt = np.arange(5)[None, :, None]; q = np.arange(128)[None, None, :]
    dist = q - k + 512 - 128 * kt
    return (np.clip(dist, -63, 256) + 63).reshape(128 * 640)


_CACHE = {}


def run(cfg, inputs, trace=False):
    L, NSEQ, S, DFF, NCORES = cfg["L"], cfg["NSEQ"], cfg["S"], cfg["DFF"], cfg["NCORES"]
    key = tuple(sorted(cfg.items()))
    if key not in _CACHE:
        _CACHE[key] = build_program(cfg)
    nc = _CACHE[key]
    f = lambda a: np.ascontiguousarray(np.asarray(a, dtype=np.float32))
    x = f(inputs["x"])
    shared = dict(
        w_in=f(inputs["w_in"]).reshape(L * D, INW),
        w_out=f(inputs["w_out"]).reshape(L * D, D),
        w_gate=f(inputs["w_gate"]).reshape(L * NE * D, DFF),
        w_up=f(inputs["w_up"]).reshape(L * NE * D, DFF),
        w_down=f(inputs["w_down"]).reshape(L * NE * DFF, D),
        bias_exp=np.ascontiguousarray(np.take(f(inputs["rel_bias"]), bias_index(), axis=2).reshape(L * 16 * 128, 640)),
        ret_gain=f(inputs["ret_norm_gain"]),
        ln1_g=f(inputs["ln1_g"]), ln1_b=f(inputs["ln1_b"]), ln2_g=f(inputs["ln2_g"]), ln2_b=f(inputs["ln2_b"]),
        router_w=f(inputs["router_w"]), router_b=f(inputs["router_b"]).reshape(1, NE),
    )
    shared.update(host_constants(cfg))
    in_maps = []
    for c in range(NCORES):
        m = dict(shared)
        m["x"] = np.ascontiguousarray(x[c * NSEQ:(c + 1) * NSEQ].reshape(NSEQ * S, D))
        in_maps.append(m)
    res = run_bass_kernel_spmd(nc, in_maps, core_ids=list(range(NCORES)), **({"trace": True} if trace else {}))
    out = np.stack([r["y"].reshape(NSEQ, S, D) for r in res.results], 0).reshape(NCORES * NSEQ, S, D)
    return out.astype(np.float32), res


def kernel(x, w_in, ret_norm_gain, rel_bias, w_out, ln1_g, ln1_b, router_w, router_b,
           w_gate, w_up, w_down, ln2_g, ln2_b):
    inputs = dict(x=x, w_in=w_in, ret_norm_gain=ret_norm_gain, rel_bias=rel_bias, w_out=w_out,
                  ln1_g=ln1_g, ln1_b=ln1_b, router_w=router_w, router_b=router_b,
                  w_gate=w_gate, w_up=w_up, w_down=w_down, ln2_g=ln2_g, ln2_b=ln2_b)
    out, _ = run(FULL, inputs)
    return out
```
